# Optimizing a Trainium2 kernel written in Bass

```python
import jax, jax.numpy as jnp
from jax import lax
import numpy as np

D_MODEL = 2048
BATCH = 2
SEQ = 4096
DEPTH = 1

CONV_CH = D_MODEL // 2
CONV_WIDTH = 31
N_HEADS = 8
HEAD_DIM = 128
ATTN_WIDTH = N_HEADS * HEAD_DIM
ROPE_DIM = HEAD_DIM // 4
ROPE_THETA = 500000.0
MOBA_BLOCK = 256
MOBA_TOPK = 3
Q_CHUNK = 32
N_GROUPS = 4
EXPERTS_PER_GROUP = 8
TOPK_IN_GROUP = 2
EXPERT_FF = D_MODEL // 4
LN_EPS = 1e-5
NEG_INF = -1e30
ALPHA = (2.0 * DEPTH) ** 0.25
BETA = (8.0 * DEPTH) ** -0.25

OFF_GLU_B = CONV_CH
OFF_Q = OFF_GLU_B + CONV_CH
OFF_K = OFF_Q + ATTN_WIDTH
OFF_V = OFF_K + ATTN_WIDTH
OFF_G_CONV = OFF_V + ATTN_WIDTH
OFF_G_ATTN = OFF_G_CONV + D_MODEL
IN_COLS = OFF_G_ATTN + D_MODEL

kernel_name = "hybrid_conformer_moba_hmoe_block"


def layer_norm(x):
    xf = x.astype(jnp.float32)
    mu = jnp.mean(xf, axis=-1, keepdims=True)
    var = jnp.mean(jnp.square(xf - mu), axis=-1, keepdims=True)
    return ((xf - mu) * lax.rsqrt(var + LN_EPS)).astype(x.dtype)


def partial_rope(x, positions):
    half = ROPE_DIM // 2
    inv_freq = jnp.power(ROPE_THETA, -jnp.arange(half, dtype=jnp.float32) / half)
    ang = positions.astype(jnp.float32)[..., None] * inv_freq
    cos = jnp.cos(ang)[:, :, None, :]
    sin = jnp.sin(ang)[:, :, None, :]
    xr = x[..., :ROPE_DIM].astype(jnp.float32)
    x1, x2 = xr[..., :half], xr[..., half:]
    rot = jnp.concatenate([x1 * cos - x2 * sin, x2 * cos + x1 * sin], axis=-1).astype(x.dtype)
    return jnp.concatenate([rot, x[..., ROPE_DIM:]], axis=-1)


def conformer_conv(a, b, w_dw, b_dw, g_cn, b_cn, w_out, b_out):
    h = a * jax.nn.sigmoid(b)
    h = lax.conv_general_dilated(
        h, w_dw[:, None, :], window_strides=(1,), padding=[(CONV_WIDTH - 1, 0)],
        dimension_numbers=("NWC", "WIO", "NWC"), feature_group_count=CONV_CH) + b_dw
    h = jax.nn.silu(layer_norm(h) * g_cn + b_cn)
    return h @ w_out + b_out


def moba_attention(q, k, v):
    B, T, H, hd = q.shape
    nb = -(-T // MOBA_BLOCK)
    tp = nb * MOBA_BLOCK
    pad = ((0, 0), (0, tp - T), (0, 0), (0, 0))
    qh = jnp.pad(q, pad).transpose(0, 2, 1, 3)
    kh = jnp.pad(k, pad).transpose(0, 2, 1, 3)
    vh = jnp.pad(v, pad).transpose(0, 2, 1, 3)
    kblk = kh.reshape(B, H, nb, MOBA_BLOCK, hd)
    vblk = vh.reshape(B, H, nb, MOBA_BLOCK, hd)
    kmean = jnp.mean(kblk.astype(jnp.float32), axis=3)
    gate = jnp.einsum('bhtd,bhnd->bhtn', qh.astype(jnp.float32), kmean)
    q_block = jnp.arange(tp) // MOBA_BLOCK
    fully_past = jnp.arange(nb)[None, :] < q_block[:, None]
    gate = jnp.where(fully_past, gate, -jnp.inf)
    n_sel = min(MOBA_TOPK, nb)
    _, sel = lax.top_k(gate, n_sel)
    scale = hd ** -0.5
    b_idx = jnp.arange(B)[:, None, None, None]
    h_idx = jnp.arange(H)[None, :, None, None]

    def step(ci):
        s = ci * Q_CHUNK
        blk = s // MOBA_BLOCK
        qc = lax.dynamic_slice_in_dim(qh, s, Q_CHUNK, axis=2)
        selc = lax.dynamic_slice_in_dim(sel, s, Q_CHUNK, axis=2)
        valid = selc < blk
        k_sel = kblk[b_idx, h_idx, selc]
        v_sel = vblk[b_idx, h_idx, selc]
        k_own = lax.dynamic_slice_in_dim(kh, blk * MOBA_BLOCK, MOBA_BLOCK, axis=2)
        v_own = lax.dynamic_slice_in_dim(vh, blk * MOBA_BLOCK, MOBA_BLOCK, axis=2)
        s_sel = jnp.einsum('bhqd,bhqjsd->bhqjs', qc, k_sel).astype(jnp.float32) * scale
        s_sel = jnp.where(valid[..., None], s_sel, NEG_INF).reshape(B, H, Q_CHUNK, n_sel * MOBA_BLOCK)
        s_own = jnp.einsum('bhqd,bhsd->bhqs', qc, k_own).astype(jnp.float32) * scale
        qpos = s + jnp.arange(Q_CHUNK)
        kpos = blk * MOBA_BLOCK + jnp.arange(MOBA_BLOCK)
        s_own = jnp.where(kpos[None, :] <= qpos[:, None], s_own, NEG_INF)
        p = jax.nn.softmax(jnp.concatenate([s_sel, s_own], axis=-1), axis=-1).astype(v.dtype)
        p_sel = p[..., :n_sel * MOBA_BLOCK].reshape(B, H, Q_CHUNK, n_sel, MOBA_BLOCK)
        p_own = p[..., n_sel * MOBA_BLOCK:]
        return (jnp.einsum('bhqjs,bhqjsd->bhqd', p_sel, v_sel)
                + jnp.einsum('bhqs,bhsd->bhqd', p_own, v_own))

    out = lax.map(step, jnp.arange(tp // Q_CHUNK))
    out = out.transpose(1, 0, 3, 2, 4).reshape(B, tp, H, hd)
    return out[:, :T]


def hier_moe(u, w_grp, b_grp, w_er, b_er, w1, w3, w2):
    B, T, D = u.shape
    uf = u.reshape(B * T, D)
    grp_logits = (uf @ w_grp).astype(jnp.float32) + b_grp.astype(jnp.float32)
    grp_prob = jax.nn.softmax(grp_logits, axis=-1)
    p_top, g_top = lax.top_k(grp_prob, 1)
    exp_logits = ((uf @ w_er).astype(jnp.float32) + b_er.astype(jnp.float32)).reshape(
        -1, N_GROUPS, EXPERTS_PER_GROUP)
    in_grp = jnp.take_along_axis(exp_logits, g_top[:, :, None], axis=1)[:, 0]
    e_val, e_idx = lax.top_k(in_grp, TOPK_IN_GROUP)
    e_w = jax.nn.softmax(e_val, axis=-1) * p_top
    exp_gate = jnp.sum(jax.nn.one_hot(e_idx, EXPERTS_PER_GROUP, dtype=jnp.float32) * e_w[..., None], axis=1)
    grp_mask = jax.nn.one_hot(g_top[:, 0], N_GROUPS, dtype=jnp.float32)
    gate = (grp_mask[:, :, None] * exp_gate[:, None, :]).astype(u.dtype)
    y = jnp.zeros_like(uf)
    for g in range(N_GROUPS):
        h = jax.nn.silu(jnp.einsum('nd,edf->nef', uf, w1[g])) * jnp.einsum('nd,edf->nef', uf, w3[g])
        y = y + jnp.einsum('nef,efd->nd', h * gate[:, g, :, None], w2[g])
    return y.reshape(B, T, D)


def setup_inputs(seed: int = 0) -> dict:
    key = jax.random.key(seed)
    ks = jax.random.split(key, 26)
    L, D, G, E, F = DEPTH, D_MODEL, N_GROUPS, EXPERTS_PER_GROUP, EXPERT_FF

    def nrm(k, shape, s):
        return jax.random.normal(k, shape, jnp.float32) * s

    col_scale = jnp.ones((IN_COLS,), jnp.float32).at[OFF_V:OFF_V + ATTN_WIDTH].set(BETA)
    positions = (jnp.arange(SEQ, dtype=jnp.int32)[None, :]
                 + jax.random.randint(ks[2], (BATCH, 1), 0, 1024, dtype=jnp.int32))
    return {
        "x": nrm(ks[0], (BATCH, SEQ, D), 1.0),
        "c": nrm(ks[1], (BATCH, D), 1.0),
        "positions": positions,
        "w_cond": nrm(ks[3], (L, D, 6 * D), 0.5 * D ** -0.5),
        "b_cond": nrm(ks[4], (L, 6 * D), 0.01),
        "w_in": nrm(ks[5], (L, D, IN_COLS), D ** -0.5) * col_scale,
        "b_glu": nrm(ks[6], (L, 2 * CONV_CH), 0.01),
        "w_dw": nrm(ks[7], (L, CONV_WIDTH, CONV_CH), CONV_WIDTH ** -0.5),
        "b_dw": nrm(ks[8], (L, CONV_CH), 0.01),
        "g_cn": 1.0 + nrm(ks[9], (L, CONV_CH), 0.02),
        "b_cn": nrm(ks[10], (L, CONV_CH), 0.01),
        "w_conv_out": nrm(ks[11], (L, CONV_CH, D), BETA * CONV_CH ** -0.5),
        "b_conv_out": nrm(ks[12], (L, D), 0.01),
        "w_attn_out": nrm(ks[13], (L, ATTN_WIDTH, D), BETA * ATTN_WIDTH ** -0.5),
        "w_mix_out": nrm(ks[14], (L, D, D), BETA * D ** -0.5),
        "g_ln1": 1.0 + nrm(ks[15], (L, D), 0.02),
        "b_ln1": nrm(ks[16], (L, D), 0.01),
        "w_grp": nrm(ks[17], (L, D, G), D ** -0.5),
        "b_grp": nrm(ks[18], (L, G), 0.01),
        "w_erouter": nrm(ks[19], (L, D, G * E), D ** -0.5),
        "b_erouter": nrm(ks[20], (L, G * E), 0.01),
        "w1": nrm(ks[21], (L, G, E, D, F), D ** -0.5),
        "w3": nrm(ks[22], (L, G, E, D, F), D ** -0.5),
        "w2": nrm(ks[23], (L, G, E, F, D), BETA * F ** -0.5),
        "g_ln2": 1.0 + nrm(ks[24], (L, D), 0.02),
        "b_ln2": nrm(ks[25], (L, D), 0.01),
    }


def reference(x, c, positions, w_cond, b_cond, w_in, b_glu, w_dw, b_dw, g_cn, b_cn,
              w_conv_out, b_conv_out, w_attn_out, w_mix_out, g_ln1, b_ln1,
              w_grp, b_grp, w_erouter, b_erouter, w1, w3, w2, g_ln2, b_ln2):
    B, T, D = x.shape
    for l in range(DEPTH):
        mod = jax.nn.silu(c) @ w_cond[l] + b_cond[l]
        sh1, sc1, gt1, sh2, sc2, gt2 = jnp.split(mod[:, None, :], 6, axis=-1)

        u = layer_norm(x) * (1 + sc1) + sh1
        z = u @ w_in[l]
        glu_a, glu_b, q, k, v, g_conv, g_attn = jnp.split(
            z, [OFF_GLU_B, OFF_Q, OFF_K, OFF_V, OFF_G_CONV, OFF_G_ATTN], axis=-1)
        ba, bb = jnp.split(b_glu[l], 2)
        y_conv = conformer_conv(glu_a + ba, glu_b + bb, w_dw[l], b_dw[l], g_cn[l], b_cn[l],
                                w_conv_out[l], b_conv_out[l])
        q = partial_rope(q.reshape(B, T, N_HEADS, HEAD_DIM), positions)
        k = partial_rope(k.reshape(B, T, N_HEADS, HEAD_DIM), positions)
        v = v.reshape(B, T, N_HEADS, HEAD_DIM)
        y_attn = moba_attention(q, k, v).reshape(B, T, ATTN_WIDTH) @ w_attn_out[l]
        merged = jax.nn.sigmoid(g_conv) * y_conv + jax.nn.sigmoid(g_attn) * y_attn
        t_out = merged @ w_mix_out[l]
        x = layer_norm(ALPHA * x + (1 + gt1) * t_out) * g_ln1[l] + b_ln1[l]

        u2 = layer_norm(x) * (1 + sc2) + sh2
        f = hier_moe(u2, w_grp[l], b_grp[l], w_erouter[l], b_erouter[l], w1[l], w3[l], w2[l])
        x = layer_norm(ALPHA * x + (1 + gt2) * f) * g_ln2[l] + b_ln2[l]
    return x
```

```python
import numpy as np
from contextlib import ExitStack
import concourse.bass as bass
import concourse.mybir as mybir
from concourse.bass_utils import run_bass_kernel_spmd

F32 = mybir.dt.float32
BF16 = mybir.dt.bfloat16
I32 = mybir.dt.int32
AF = mybir.ActivationFunctionType
ALU = mybir.AluOpType

D = 2048
SEQ = 4096
NT = 1024
NCTX = 3072
NKEY = NCTX + NT
CONV_CH = 1024
CW = 31
NH = 8
HD = 128
IN_COLS = 9216
OFF_Q = 2048
OFF_K = 3072
OFF_V = 4096
OFF_GC = 5120
OFF_GA = 7168
NEXP = 32
FF = 512
EPS = 1e-5
ALPHA = 2.0 ** 0.25
SCALE = HD ** -0.5
TWO_PI = 2.0 * np.pi
MASKV = 3.0e4

VOFF = {}
_o = 0
for _n, _w in [("bglu", 16), ("bdw", 8), ("gcn", 8), ("bcn", 8), ("bco", 16), ("gln1", 16),
               ("bln1", 16), ("gln2", 16), ("bln2", 16), ("bcond", 96), ("c", 16), ("halo", 1),
               ("invf", 1)]:
    VOFF[_n] = (_o, _w)
    _o += _w
NV = _o


class Buf:
    __slots__ = ("w", "r")

    def __init__(self):
        self.w = None
        self.r = {}


class Sched:
    def __init__(self, nc, stack, nlanes=6):
        self.nc = nc
        self.E = {}
        for name, h in [("pe", nc.tensor), ("act", nc.scalar), ("dve", nc.vector),
                        ("pool", nc.gpsimd), ("sp", nc.sync)]:
            sem = stack.enter_context(nc.semaphore("s_" + name))
            self.E[name] = dict(h=h, sem=sem, cnt=0, waited={}, name=name)
        self.lanes = {}
        for q in ("sp", "pool"):
            self.lanes[q] = [dict(sem=stack.enter_context(nc.semaphore(f"l_{q}{i}")), cnt=0)
                             for i in range(nlanes)]
        self.rr = {q: 0 for q in self.lanes}
        self.nwaits = 0
        self.nops = 0

    def _wait(self, eng, ev):
        sem, val = ev
        e = self.E[eng]
        if sem is e["sem"] and eng == "pe":
            return
        k = id(sem)
        if e["waited"].get(k, 0) >= val:
            return
        e["h"].wait_ge(sem, val)
        e["waited"][k] = val
        self.nwaits += 1

    def _deps(self, eng, reads, writes):
        for b in reads:
            if b.w is not None:
                self._wait(eng, b.w)
        for b in writes:
            if b.w is not None:
                self._wait(eng, b.w)
            for ev in b.r.values():
                self._wait(eng, ev)

    def _commit(self, ev, reads, writes):
        k = id(ev[0])
        for b in reads:
            old = b.r.get(k)
            if old is None or old[1] < ev[1]:
                b.r[k] = ev
        for b in writes:
            b.w = ev
            b.r = {}

    def op(self, eng, fn, reads=(), writes=()):
        e = self.E[eng]
        self._deps(eng, reads, writes)
        ins = fn(e["h"])
        e["cnt"] += 1
        ins.then_inc(e["sem"], 1)
        ev = (e["sem"], e["cnt"])
        self._commit(ev, reads, writes)
        self.nops += 1
        return ev

    def dma(self, q, out, in_, reads=(), writes=()):
        e = self.E[q]
        lanes = self.lanes[q]
        ln = lanes[self.rr[q] % len(lanes)]
        self.rr[q] += 1
        self._deps(q, reads, writes)
        if ln["cnt"] > 0:
            self._wait(q, (ln["sem"], 16 * ln["cnt"]))
        ins = e["h"].dma_start(out=out, in_=in_)
        ln["cnt"] += 1
        ins.then_inc(ln["sem"], 16)
        ev = (ln["sem"], 16 * ln["cnt"])
        self._commit(ev, reads, writes)
        self.nops += 1
        return ev

    def all_events(self):
        evs = []
        for e in self.E.values():
            if e["cnt"]:
                evs.append((e["sem"], e["cnt"]))
        for lanes in self.lanes.values():
            for ln in lanes:
                if ln["cnt"]:
                    evs.append((ln["sem"], 16 * ln["cnt"]))
        return evs

    def barrier(self, engines=("pe", "act", "dve", "pool", "sp")):
        evs = self.all_events()
        for eng in engines:
            for ev in evs:
                self._wait(eng, ev)


def build_program(stage=99, dbg=False):
    nc = bass.Bass("TRN2", target_bir_lowering=False)

    def din(name, shape, dt=F32):
        return nc.dram_tensor(name, list(shape), dt, kind="ExternalInput").ap()

    x_own = din("x_own", [NT, D])
    x_halo = din("x_halo", [128, D])
    x_ctx = din("x_ctx", [NCTX, D])
    pos_in = din("pos", [32, NKEY], I32)
    vecs_in = din("vecs", [128, NV])
    negmask_in = din("negmask", [128, 128])
    force_in = din("force", [128, 128])
    wdwT_in = din("wdwT", [128, 8 * CW])
    brow_in = din("brow", [128, 36])
    wr_in = din("wr", [128, 16 * 36])
    prot_in = din("prot", [32, 32])
    blksel_in = din("blksel", [16, 16 * 128])
    w_cond = din("w_cond", [D, 6 * D])
    w_in = din("w_in", [D, IN_COLS])
    w_conv_out = din("w_conv_out", [CONV_CH, D])
    w_attn_out = din("w_attn_out", [NH * HD, D])
    w_mix_out = din("w_mix_out", [D, D])
    w1 = din("w1", [NEXP, D, FF])
    w3 = din("w3", [NEXP, D, FF])
    w2 = din("w2", [NEXP, FF, D])
    out = nc.dram_tensor("out", [NT, D], F32, kind="ExternalOutput").ap()

    Kt_d = nc.dram_tensor("Kt_d", [NH, 128, NKEY], BF16, kind="Internal").ap()
    V_d = nc.dram_tensor("V_d", [NKEY, NH * HD], BF16, kind="Internal").ap()
    x1_d = nc.dram_tensor("x1_d", [128, 16 * NT], F32, kind="Internal").ap()
    gate_d = nc.dram_tensor("gate_d", [NEXP, NT], F32, kind="Internal").ap()

    dbg_outs = {}

    with ExitStack() as top:
        S = Sched(nc, top)

        uid = [0]

        def sb(st, name, shape, dt):
            uid[0] += 1
            return st.enter_context(nc.sbuf_tensor(f"s{uid[0]}_{name}", list(shape), dt))

        def ps(st, name, shape, dt=F32):
            uid[0] += 1
            return st.enter_context(nc.psum_tensor(f"p{uid[0]}_{name}", list(shape), dt))

        def dump(name, src_ap, shape, dt=F32, reads=()):
            if not dbg:
                return
            t = nc.dram_tensor("dbg_" + name, list(shape), dt, kind="ExternalOutput").ap()
            S.dma("sp", t, src_ap, reads=list(reads), writes=[Buf()])
            dbg_outs[name] = (list(shape), dt)

        def finish():
            S.barrier(engines=("sp",))

        vecs = sb(top, "vecs", [128, NV], F32); b_vecs = Buf()
        identf = sb(top, "identf", [128, 128], F32); b_idf = Buf()
        identb = sb(top, "identb", [128, 128], BF16); b_idb = Buf()
        onesD = sb(top, "onesD", [128, 128], F32); b_onesD = Buf()
        onesC = sb(top, "onesC", [128, 128], F32); b_onesC = Buf()
        onesb = sb(top, "onesb", [128, 128], BF16); b_onesb = Buf()
        modT = sb(top, "modT", [128, 96], F32); b_modT = Buf()
        opm = sb(top, "opm", [128, 96], F32); b_opm = Buf()
        ring = [(sb(top, f"ring{i}", [128, 16, 512], BF16), Buf()) for i in range(4)]
        ring_pos = [0]

        def V(name, c0=0, c1=None):
            o, w = VOFF[name]
            if c1 is None:
                c1 = c0 + 1
            return vecs[:, o + c0:o + c1]

        S.dma("sp", vecs[:], vecs_in[:, :], writes=[b_vecs])
        S.op("pool", lambda e: e.memset(identf[:], 0.0), writes=[b_idf])
        S.op("pool", lambda e: e.affine_select(out=identf[:], in_=identf[:], pattern=[[-1, 128]],
                                               compare_op=ALU.not_equal, fill=1.0, base=0,
                                               channel_multiplier=1), reads=[b_idf], writes=[b_idf])
        S.op("dve", lambda e: e.tensor_copy(out=identb[:], in_=identf[:]), reads=[b_idf], writes=[b_idb])
        S.op("dve", lambda e: e.memset(onesD[:], 1.0 / D), writes=[b_onesD])
        S.op("dve", lambda e: e.memset(onesC[:], 1.0 / CONV_CH), writes=[b_onesC])
        S.op("dve", lambda e: e.memset(onesb[:], 1.0), writes=[b_onesb])

        class WStream:
            def __init__(self, blocks, nslots=4, lookahead=2):
                self.blocks = blocks
                self.issued = 0
                self.la = lookahead
                self.nslots = nslots
                self.slots = {}

            def _issue(self, i):
                slot, b = ring[i % self.nslots]
                for fn, src in self.blocks[i]:
                    S.dma("pool", fn(slot), src, writes=[b])
                self.slots[i] = (slot, b)

            def get(self, i):
                while self.issued < min(len(self.blocks), i + 1 + self.la):
                    self._issue(self.issued)
                    self.issued += 1
                return self.slots[i]

        def colblock(w_ap, c0, ncols=512, kc=16):
            return [(lambda s: s[:, 0:kc, 0:ncols],
                     w_ap[:, c0:c0 + ncols].rearrange("(c p) n -> p c n", p=128))]

        csb = sb(top, "csb", [128, 16], BF16); b_csb = Buf()
        S.op("act", lambda e: e.activation(out=csb[:], in_=V("c", 0, 16), func=AF.Silu), reads=[b_vecs], writes=[b_csb])
        with ExitStack() as st:
            pmod = ps(st, "pmod", [128, 32]); b_pmod = Buf()
            ws = WStream([colblock(w_cond, i * 512) for i in range(8)], nslots=4, lookahead=2)
            for i in range(8):
                slot, bslot = ws.get(i)
                for cc in range(4):
                    q = i * 4 + cc
                    for k in range(16):
                        S.op("pe", lambda e: e.matmul(pmod[:, q:q + 1], lhsT=slot[:, k, cc * 128:(cc + 1) * 128],
                                                      rhs=csb[:, k:k + 1], start=(k == 0), stop=(k == 15)),
                             reads=[bslot, b_csb], writes=[b_pmod])
            S.op("dve", lambda e: e.tensor_tensor(out=modT[:, 0:32], in0=pmod[:, 0:32], in1=V("bcond", 0, 32), op=ALU.add),
                 reads=[b_pmod, b_vecs], writes=[b_modT])
            S.op("dve", lambda e: e.tensor_scalar(out=opm[:, 0:32], in0=modT[:, 0:32], scalar1=1.0, scalar2=None, op0=ALU.add),
                 reads=[b_modT], writes=[b_opm])
            S.barrier()
        if stage <= 0:
            finish()
            return nc, dbg_outs

        SH1, SC1, GT1, SH2, SC2, GT2 = 0, 16, 32, 48, 64, 80

        def ln_tile_to_uT(st_tag, xt, b_xt, work, dst_fn, b_dst, pT, b_pT):
            stats, mv, rstd, nmr, xn, b_w = work
            for s4 in range(4):
                S.op("dve", lambda e: e.bn_stats(out=stats[:, s4, :], in_=xt[:, s4 * 512:(s4 + 1) * 512]),
                     reads=[b_xt], writes=[b_w])
            S.op("dve", lambda e: e.bn_aggr(out=mv[:], in_=stats[:].rearrange("p a b -> p (a b)")),
                 reads=[b_w], writes=[b_w])
            S.op("dve", lambda e: e.tensor_scalar(out=rstd[:], in0=mv[:, 1:2], scalar1=EPS, scalar2=None,
                                                  op0=ALU.add), reads=[b_w], writes=[b_w])
            S.op("act", lambda e: e.activation(out=rstd[:], in_=rstd[:], func=AF.Sqrt), reads=[b_w], writes=[b_w])
            S.op("dve", lambda e: e.reciprocal(out=rstd[:], in_=rstd[:]), reads=[b_w], writes=[b_w])
            S.op("dve", lambda e: e.scalar_tensor_tensor(out=nmr[:], in0=mv[:, 0:1], scalar=-1.0, in1=rstd[:],
                                                         op0=ALU.mult, op1=ALU.mult), reads=[b_w], writes=[b_w])
            S.op("act", lambda e: e.activation(out=xn[:], in_=xt[:], func=AF.Identity, bias=nmr[:], scale=rstd[:]),
                 reads=[b_xt, b_w], writes=[b_w])
            for c in range(16):
                S.op("pe", lambda e: e.transpose(out=pT[:, c * 128:(c + 1) * 128], in_=xn[:, c * 128:(c + 1) * 128],
                                                 identity=identb[:]), reads=[b_w, b_idb], writes=[b_pT])
            for c in range(16):
                if c % 2 == 0:
                    S.op("dve", lambda e: e.tensor_scalar(out=dst_fn(c), in0=pT[:, c * 128:(c + 1) * 128],
                                                          scalar1=opm[:, SC1 + c:SC1 + c + 1],
                                                          scalar2=modT[:, SH1 + c:SH1 + c + 1],
                                                          op0=ALU.mult, op1=ALU.add),
                         reads=[b_pT, b_opm, b_modT], writes=[b_dst])
                else:
                    S.op("act", lambda e: e.activation(out=dst_fn(c), in_=pT[:, c * 128:(c + 1) * 128],
                                                       func=AF.Identity, scale=opm[:, SC1 + c:SC1 + c + 1],
                                                       bias=modT[:, SH1 + c:SH1 + c + 1]),
                         reads=[b_pT, b_opm, b_modT], writes=[b_dst])

        def rope_rows(src_ps, b_src, cs_ap, sn_ap, dst_ap, b_dst, tmp, b_tmp, prot, b_prot, prot_ps, b_prps, n, extra_reads=()):
            kr, t1 = tmp
            S.op("act", lambda e: e.activation(out=kr[0:32, 0:n], in_=src_ps[0:32, 0:n], func=AF.Identity),
                 reads=[b_src], writes=[b_tmp])
            S.op("pe", lambda e: e.matmul(prot_ps[0:32, 0:n], lhsT=prot[0:32, 0:32], rhs=kr[0:32, 0:n],
                                          start=True, stop=True), reads=[b_tmp, b_prot], writes=[b_prps])
            S.op("dve", lambda e: e.tensor_tensor(out=t1[0:32, 0:n], in0=kr[0:32, 0:n], in1=cs_ap, op=ALU.mult),
                 reads=[b_tmp] + list(extra_reads), writes=[b_tmp])
            S.op("dve", lambda e: e.tensor_tensor(out=kr[0:32, 0:n], in0=prot_ps[0:32, 0:n], in1=sn_ap, op=ALU.mult),
                 reads=[b_prps, b_tmp] + list(extra_reads), writes=[b_tmp])
            S.op("dve", lambda e: e.tensor_tensor(out=dst_ap, in0=kr[0:32, 0:n], in1=t1[0:32, 0:n], op=ALU.add),
                 reads=[b_tmp], writes=[b_dst])

        AX = mybir.AxisListType.X
        mu = sb(top, "mu", [128, 16, NT], BF16)
        b_mu = [Buf(), Buf()]
        mu_f = mu[:].bitcast(F32).rearrange("p a b -> p (a b)")
        sT = ring[3][0][:].rearrange("p a b -> p (a b)").rearrange("p (c t) -> p c t", c=8)
        b_sT = Buf()
        sub1 = ExitStack()
        uT = sb(sub1, "uT", [128, 16, NT], BF16)
        b_uT = [Buf(), Buf()]
        cso = sb(sub1, "cso", [32, NT], F32)
        sno = sb(sub1, "sno", [32, NT], F32)
        b_tabo = [Buf(), Buf()]
        kmean = sb(sub1, "kmean", [128, NH, 16], F32); b_kmean = Buf()
        kmax2 = sb(sub1, "kmax2", [128, NH], F32); b_kmax2 = Buf()
        prot = sb(sub1, "prot", [32, 32], F32); b_prot = Buf()
        attnT = sb(sub1, "attnT", [128, NH, NT], BF16); b_attnT = [Buf() for _ in range(NH)]
        S.dma("sp", prot[:], prot_in[:, :], writes=[b_prot])

        def rope_tables(col0, n, cs_ap, sn_ap, b_tab, tmps, b_tmp, deferred=None, ops_out=None):
            posi, yv, fv_s, ki, mk, fv_c = tmps
            ops = []

            def D(fn):
                ops.append(lambda: S.op("dve", fn, reads=[b_tmp, b_vecs], writes=[b_tmp]))

            ops.append(lambda: S.dma("sp", posi[:, 0:n], pos_in[:, col0:col0 + n], writes=[b_tmp]))
            D(lambda e: e.tensor_copy(out=yv[:, 0:n], in_=posi[:, 0:n]))
            D(lambda e: e.tensor_scalar(out=yv[:, 0:n], in0=yv[:, 0:n], scalar1=V("invf")[0:32, :],
                                        scalar2=float(1.0 / TWO_PI), op0=ALU.mult, op1=ALU.mult))
            sins = []
            for which, dst, fv in (("sin", sn_ap, fv_s), ("cos", cs_ap, fv_c)):
                if which == "cos":
                    D(lambda e: e.tensor_scalar(out=yv[:, 0:n], in0=yv[:, 0:n], scalar1=0.25, scalar2=None, op0=ALU.add))
                D(lambda e, fv=fv: e.tensor_copy(out=ki[:, 0:n], in_=yv[:, 0:n]))
                D(lambda e, fv=fv: e.tensor_copy(out=fv[:, 0:n], in_=ki[:, 0:n]))
                D(lambda e, fv=fv: e.tensor_tensor(out=fv[:, 0:n], in0=yv[:, 0:n], in1=fv[:, 0:n], op=ALU.subtract))
                D(lambda e, fv=fv: e.tensor_scalar(out=mk[:, 0:n], in0=fv[:, 0:n], scalar1=0.5, scalar2=None, op0=ALU.is_gt))
                D(lambda e, fv=fv: e.tensor_tensor(out=fv[:, 0:n], in0=fv[:, 0:n], in1=mk[:, 0:n], op=ALU.subtract))
                D(lambda e, fv=fv: e.tensor_scalar(out=mk[:, 0:n], in0=fv[:, 0:n], scalar1=-0.5, scalar2=None, op0=ALU.is_lt))
                D(lambda e, fv=fv: e.tensor_tensor(out=fv[:, 0:n], in0=fv[:, 0:n], in1=mk[:, 0:n], op=ALU.add))

                def sin_op(dst=dst, fv=fv):
                    S.op("act", lambda e: e.activation(out=dst, in_=fv[:, 0:n], func=AF.Sin, scale=float(TWO_PI)),
                         reads=[b_tmp], writes=[b_tab])
                sins.append(sin_op)
            if ops_out is None:
                for o in ops:
                    o()
            else:
                ops_out.extend(ops)
            if deferred is None:
                for f_ in sins:
                    f_()
            else:
                deferred.extend(sins)

        def ln_stats(src, b_src, C, ones, b_ones, meanb, rstdb, b_stat, sqr, mp, b_mp, ep, b_ep, msq, b_msq):
            for half in range(2):
                hs = slice(half * 512, (half + 1) * 512)
                for c in range(C):
                    sq, b_sq = sqr[c % 2]
                    S.op("act", lambda e: e.activation(out=sq[:, :], in_=src(c, half), func=AF.Square),
                         reads=[b_src(c)], writes=[b_sq])
                    S.op("pe", lambda e: e.matmul(mp[:, :], lhsT=ones[:, :], rhs=src(c, half), start=(c == 0),
                                                  stop=(c == C - 1)), reads=[b_src(c), b_ones], writes=[b_mp])
                    S.op("pe", lambda e: e.matmul(ep[:, :], lhsT=ones[:, :], rhs=sq[:, :], start=(c == 0),
                                                  stop=(c == C - 1)), reads=[b_sq, b_ones], writes=[b_ep])
                S.op("act", lambda e: e.activation(out=meanb[:, hs], in_=mp[:, :], func=AF.Identity),
                     reads=[b_mp], writes=[b_stat])
                S.op("act", lambda e: e.activation(out=msq[:, :], in_=mp[:, :], func=AF.Square),
                     reads=[b_mp], writes=[b_msq])
                S.op("dve", lambda e: e.tensor_tensor(out=rstdb[:, hs], in0=ep[:, :], in1=msq[:, :], op=ALU.subtract),
                     reads=[b_ep, b_msq], writes=[b_stat])
                S.op("dve", lambda e: e.tensor_scalar(out=rstdb[:, hs], in0=rstdb[:, hs], scalar1=0.0, scalar2=EPS,
                                                      op0=ALU.max, op1=ALU.add), reads=[b_stat], writes=[b_stat])
                S.op("act", lambda e: e.activation(out=rstdb[:, hs], in_=rstdb[:, hs], func=AF.Sqrt),
                     reads=[b_stat], writes=[b_stat])
                S.op("dve", lambda e: e.reciprocal(out=rstdb[:, hs], in_=rstdb[:, hs]), reads=[b_stat], writes=[b_stat])

        with ExitStack() as st, ExitStack() as pst:
            wk0, bwk0 = ring[0]
            wk1, bwk1 = ring[1]
            wv0, bwv0 = ring[2]
            wv1, bwv1 = ring[3]
            for (slot, bslot), c0 in ((ring[0], OFF_K), (ring[1], OFF_K + 512), (ring[2], OFF_V), (ring[3], OFF_V + 512)):
                S.dma("pool", slot[:, :, :], w_in[:, c0:c0 + 512].rearrange("(c p) n -> p c n", p=128), writes=[bslot])
            xts = [(sb(st, f"xt{i}", [128, D], F32), Buf()) for i in range(2)]
            works = []
            for i in range(4):
                works.append((sb(st, f"stats{i}", [128, 4, 6], F32), sb(st, f"mv{i}", [128, 2], F32),
                              sb(st, f"rstd{i}", [128, 1], F32), sb(st, f"nmr{i}", [128, 1], F32),
                              sb(st, f"xn{i}", [128, D], BF16), Buf()))
            pT, b_pT = ps(pst, "pT", [128, D], BF16), Buf()
            banks = [(ps(pst, f"kvb{i}", [128, 512]), Buf()) for i in range(4)]
            prps = (ps(pst, "prps", [32, 512]), Buf())
            nrmb = (ps(pst, "nrmb", [128, 512]), Buf())
            ktsb = [(sb(st, f"ktsb{i}", [128, 512], BF16), Buf()) for i in range(3)]
            ksq = [(sb(st, f"ksq{i}", [128, 512], BF16), Buf()) for i in range(1)]
            vsb = [(sb(st, f"vsb{i}", [128, 1024], BF16), Buf()) for i in range(2)]
            ascr = attnT[:].bitcast(F32)
            rtmp = [((ascr[0:32, 2 * i, :], ascr[0:32, 2 * i + 1, :]), Buf()) for i in range(2)]
            tabs = [((ascr[0:32, 4 + 2 * i, :], ascr[0:32, 5 + 2 * i, :]), Buf()) for i in range(2)]
            pk = sb(st, "posi", [32, 512], I32)
            ttmp = (pk, sb(st, "yv", [32, 512], F32), sb(st, "fv", [32, 512], F32), pk, sb(st, "mk", [32, 512], F32),
                    sb(st, "fvc", [32, 512], F32))
            b_ttmp = Buf()
            kred = sb(st, "kred", [128, 2], F32); b_kred = Buf()
            S.op("dve", lambda e: e.memset(kmax2[:], 0.0), writes=[b_kmax2])
            nb = [0]

            def grp(g):
                own = g >= 6
                if own:
                    ug_buf = b_uT[g - 6]
                    ug = lambda c, t0, n: uT[:, c, (g - 6) * 512 + t0:(g - 6) * 512 + t0 + n]
                    cs_g, sn_g, b_tab = cso[:, (g - 6) * 512:(g - 5) * 512], sno[:, (g - 6) * 512:(g - 5) * 512], b_tabo[g - 6]
                else:
                    ug_buf = b_mu[g % 2]
                    ug = lambda c, t0, n: mu[:, c, (g % 2) * 512 + t0:(g % 2) * 512 + t0 + n]
                    (cs_t, sn_t), b_tab = tabs[g % 2]
                    cs_g, sn_g = cs_t, sn_t
                return own, ug, ug_buf, cs_g, sn_g, b_tab

            def ln_pre(g, t):
                own = g >= 6
                idx = g * 4 + t
                xt, b_xt = xts[idx % 2]
                stats, mv, rstd, nmr, xn, b_w = works[idx % 4]
                if own:
                    src = x_own[(g - 6) * 512 + t * 128:(g - 6) * 512 + (t + 1) * 128, :]
                else:
                    src = x_ctx[g * 512 + t * 128:g * 512 + (t + 1) * 128, :]
                S.dma("sp", xt[:], src, writes=[b_xt])
                for s4 in range(4):
                    S.op("dve", lambda e: e.bn_stats(out=stats[:, s4, :], in_=xt[:, s4 * 512:(s4 + 1) * 512]),
                         reads=[b_xt], writes=[b_w])
                S.op("dve", lambda e: e.bn_aggr(out=mv[:], in_=stats[:].rearrange("p a b -> p (a b)")),
                     reads=[b_w], writes=[b_w])
                S.op("dve", lambda e: e.tensor_scalar(out=rstd[:], in0=mv[:, 1:2], scalar1=EPS, scalar2=None,
                                                      op0=ALU.add), reads=[b_w], writes=[b_w])

            def ln_pre_b(g, t):
                idx = g * 4 + t
                xt, b_xt = xts[idx % 2]
                stats, mv, rstd, nmr, xn, b_w = works[idx % 4]
                S.op("act", lambda e: e.activation(out=rstd[:], in_=rstd[:], func=AF.Sqrt), reads=[b_w], writes=[b_w])
                S.op("dve", lambda e: e.reciprocal(out=rstd[:], in_=rstd[:]), reads=[b_w], writes=[b_w])
                S.op("dve", lambda e: e.scalar_tensor_tensor(out=nmr[:], in0=mv[:, 0:1], scalar=-1.0, in1=rstd[:],
                                                             op0=ALU.mult, op1=ALU.mult), reads=[b_w], writes=[b_w])
                S.op("act", lambda e: e.activation(out=xn[:], in_=xt[:], func=AF.Identity, bias=nmr[:], scale=rstd[:]),
                     reads=[b_xt, b_w], writes=[b_w])

            def ln_pe(g, t):
                own, ug, ug_buf, _, _, _ = grp(g)
                idx = g * 4 + t
                stats, mv, rstd, nmr, xn, b_w = works[idx % 4]
                for c in range(16):
                    S.op("pe", lambda e: e.transpose(out=pT[:, c * 128:(c + 1) * 128], in_=xn[:, c * 128:(c + 1) * 128],
                                                     identity=identb[:]), reads=[b_w, b_idb], writes=[b_pT])
                for c in range(16):
                    dst = ug(c, t * 128, 128)
                    if c % 2 == 0:
                        S.op("dve", lambda e: e.tensor_scalar(out=dst, in0=pT[:, c * 128:(c + 1) * 128],
                                                              scalar1=opm[:, SC1 + c:SC1 + c + 1],
                                                              scalar2=modT[:, SH1 + c:SH1 + c + 1],
                                                              op0=ALU.mult, op1=ALU.add),
                             reads=[b_pT, b_opm, b_modT], writes=[ug_buf])
                    else:
                        S.op("act", lambda e: e.activation(out=dst, in_=pT[:, c * 128:(c + 1) * 128],
                                                           func=AF.Identity, scale=opm[:, SC1 + c:SC1 + c + 1],
                                                           bias=modT[:, SH1 + c:SH1 + c + 1]),
                             reads=[b_pT, b_opm, b_modT], writes=[ug_buf])

            def stage_A(g, h, kb, b_kb):
                own, ug, ug_buf, cs_g, sn_g, b_tab = grp(g)
                hh = g * NH + h
                kt, b_kt = ktsb[hh % 3]
                (kr, t1), b_tmp = rtmp[hh % 2]
                prp, b_prp = prps
                S.op("pe", lambda e: e.matmul(prp[0:32, :], lhsT=prot[0:32, 0:32], rhs=kr, start=True, stop=True),
                     reads=[b_tmp, b_prot], writes=[b_prp])
                S.op("pool", lambda e: e.tensor_tensor(out=t1, in0=kr, in1=cs_g, op=ALU.mult),
                     reads=[b_tmp, b_tab], writes=[b_tmp])
                S.op("dve", lambda e: e.tensor_tensor(out=kr, in0=prp[0:32, :], in1=sn_g, op=ALU.mult),
                     reads=[b_prp, b_tmp, b_tab], writes=[b_tmp])
                S.op("pool", lambda e: e.tensor_tensor(out=kt[0:32, :], in0=kr, in1=t1, op=ALU.add),
                     reads=[b_tmp], writes=[b_kt])
                S.dma("sp", Kt_d[h, :, g * 512:(g + 1) * 512], kt[:, :], reads=[b_kt], writes=[Buf()])
                S.op("dve", lambda e: e.tensor_reduce(out=kmean[:, h, 2 * g:2 * g + 2],
                                                      in_=kt[:, :].rearrange("p (b t) -> p b t", b=2),
                                                      axis=AX, op=ALU.add), reads=[b_kt], writes=[b_kmean])
                sq, b_sq = ksq[0]
                S.op("act", lambda e: e.activation(out=sq[:, :], in_=kt[:, :], func=AF.Square),
                     reads=[b_kt], writes=[b_sq])

            def stage_B(g, h):
                hh = g * NH + h
                sq, b_sq = ksq[0]
                nbk, b_nbk = nrmb
                S.op("pe", lambda e: e.matmul(nbk[:, :], lhsT=onesb[:, :], rhs=sq[:, :], start=True, stop=True),
                     reads=[b_sq, b_onesb], writes=[b_nbk])
                S.op("dve", lambda e: e.tensor_reduce(out=kred[:, 0:1], in_=nbk[:, :], axis=AX, op=ALU.max),
                     reads=[b_nbk], writes=[b_kred])
                S.op("dve", lambda e: e.tensor_tensor(out=kmax2[:, h:h + 1], in0=kmax2[:, h:h + 1],
                                                      in1=kred[:, 0:1], op=ALU.max),
                     reads=[b_kred, b_kmax2], writes=[b_kmax2])

            pendA = []
            pendB = []

            def pump():
                if pendB:
                    stage_B(*pendB.pop(0))
                if pendA:
                    a_ = pendA.pop(0)
                    stage_A(*a_)
                    pendB.append((a_[0], a_[1]))

            own0, _, _, cs0, sn0, bt0 = grp(0)
            rope_tables(0, 512, cs0, sn0, bt0, ttmp, b_ttmp)
            for t in range(4):
                ln_pre(0, t)
                ln_pre_b(0, t)
                ln_pe(0, t)
            for g in range(8):
                own, ug, ug_buf, cs_g, sn_g, b_tab = grp(g)
                sin_def = []
                rope_ops = []
                if g + 1 < 8:
                    _, _, _, cs_n, sn_n, bt_n = grp(g + 1)
                    rope_tables((g + 1) * 512, 512, cs_n, sn_n, bt_n, ttmp, b_ttmp, deferred=sin_def, ops_out=rope_ops)
                for h in range(NH):
                    hh = g * NH + h
                    wslot, bws = (wk0, bwk0) if h < 4 else (wk1, bwk1)
                    kb, b_kb = banks[nb[0] % 4]; nb[0] += 1
                    for k in range(16):
                        S.op("pe", lambda e: e.matmul(kb[:, :], lhsT=wslot[:, k, (h % 4) * 128:(h % 4 + 1) * 128],
                                                      rhs=ug(k, 0, 512), start=(k == 0), stop=(k == 15)),
                             reads=[bws, ug_buf], writes=[b_kb])
                    kt, b_kt = ktsb[hh % 3]
                    (kr, t1), b_tmp = rtmp[hh % 2]
                    S.op("act", lambda e: e.activation(out=kt[:, :], in_=kb[:, :], func=AF.Identity),
                         reads=[b_kb], writes=[b_kt])
                    S.op("act", lambda e: e.activation(out=kr, in_=kb[0:32, :], func=AF.Identity),
                         reads=[b_kb], writes=[b_tmp])
                    pump()
                    pendA.append((g, h, kb, b_kb))
                    for _ in range(4):
                        if rope_ops:
                            rope_ops.pop(0)()
                    if h == 7:
                        for f_ in sin_def:
                            f_()
                    if g + 1 < 8:
                        if 1 <= h <= 4:
                            ln_pre_b(g + 1, h - 1)
                        if h < 4:
                            ln_pre(g + 1, h)
                        else:
                            ln_pe(g + 1, h - 4)
                for t in range(4):
                    vt, b_vt = vsb[t % 2]
                    for cb in range(2):
                        wslot, bws = (wv0, bwv0) if cb == 0 else (wv1, bwv1)
                        vb, b_vb = banks[nb[0] % 4]; nb[0] += 1
                        for k in range(16):
                            S.op("pe", lambda e: e.matmul(vb[:, :], lhsT=ug(k, t * 128, 128), rhs=wslot[:, k, :],
                                                          start=(k == 0), stop=(k == 15)),
                                 reads=[bws, ug_buf], writes=[b_vb])
                        if cb == 0:
                            S.op("act", lambda e: e.activation(out=vt[:, 0:512], in_=vb[:, :], func=AF.Identity),
                                 reads=[b_vb], writes=[b_vt])
                        else:
                            S.op("dve", lambda e: e.tensor_copy(out=vt[:, 512:1024], in_=vb[:, :]),
                                 reads=[b_vb], writes=[b_vt])
                    S.dma("sp", V_d[g * 512 + t * 128:g * 512 + (t + 1) * 128, :], vt[:, :], reads=[b_vt],
                          writes=[Buf()])
                    if t < 2:
                        pump()
            while pendA or pendB:
                pump()
            S.barrier()
        if stage <= 1:
            sub1.close()
            finish()
            return nc, dbg_outs

        with ExitStack() as st:
            hT = sb(st, "hT", [128, 8, 30 + NT], BF16); b_hT = [Buf() for _ in range(8)]
            with ExitStack() as st2, ExitStack() as pst:
                uTh = sb(st2, "uTh", [128, 16, 128], BF16); b_uTh = Buf()
                xt = sb(st2, "xth", [128, D], F32); b_xt = Buf()
                work = (sb(st2, "statsh", [128, 4, 6], F32), sb(st2, "mvh", [128, 2], F32),
                        sb(st2, "rstdh", [128, 1], F32), sb(st2, "nmrh", [128, 1], F32),
                        sb(st2, "xnh", [128, D], BF16), Buf())
                pT = ps(pst, "pTh", [128, D], BF16); b_pT = Buf()
                pab = [(ps(pst, f"pab{i}", [128, 512]), Buf()) for i in range(4)]
                sgt = [(sb(st2, f"sgt{i}", [128, 512], F32), Buf()) for i in range(2)]
                hh = sb(st2, "hh", [128, 128], F32); b_hh = Buf()
                S.dma("sp", xt[:], x_halo[:, :], writes=[b_xt])
                ln_tile_to_uT("halo", xt, b_xt, work, lambda c: uTh[:, c, :], b_uTh, pT, b_pT)
                blocks = []
                for i in range(4):
                    blocks.append([
                        (lambda s: s[:, :, 0:256], w_in[:, 256 * i:256 * i + 256].rearrange("(c p) n -> p c n", p=128)),
                        (lambda s: s[:, :, 256:512],
                         w_in[:, 1024 + 256 * i:1024 + 256 * i + 256].rearrange("(c p) n -> p c n", p=128))])
                ws = WStream(blocks, nslots=3)
                npb = 0
                pmod2 = ps(pst, "pmod2", [128, 64]); b_pmod2 = Buf()
                r3 = ring[3][0]
                wcs = [(r3[:, :, 0:256], Buf()), (r3[:, :, 256:512], Buf())]
                mstate = {"issued": 0, "done": 0}
                NMB = 32

                def mod_issue():
                    bi = mstate["issued"]
                    if bi >= NMB:
                        return
                    wt, b_wt = wcs[bi % 2]
                    c0 = 4096 + bi * 256
                    S.dma("pool", wt, w_cond[:, c0:c0 + 256].rearrange("(c p) n -> p c n", p=128), writes=[b_wt])
                    mstate["issued"] += 1

                def mod_compute():
                    bi = mstate["done"]
                    if bi >= mstate["issued"]:
                        return
                    wt, b_wt = wcs[bi % 2]
                    for c2 in range(2):
                        q = 2 * bi + c2
                        for k in range(16):
                            S.op("pe", lambda e: e.matmul(pmod2[:, q:q + 1], lhsT=wt[:, k, c2 * 128:(c2 + 1) * 128],
                                                          rhs=csb[:, k:k + 1], start=(k == 0), stop=(k == 15)),
                                 reads=[b_wt, b_csb], writes=[b_pmod2])
                    mstate["done"] += 1

                mod_issue()
                mod_issue()
                unit = 0
                for i in range(4):
                    slot, bslot = ws.get(i)
                    for cc in range(2):
                        ch = 2 * i + cc
                        for seg in range(3):
                            n = 512 if seg < 2 else 128
                            pa, b_pa = pab[npb % 4]; npb += 1
                            pb, b_pb = pab[npb % 4]; npb += 1
                            if seg < 2:
                                rhs_fn = lambda k: uT[:, k, seg * 512:(seg + 1) * 512]
                                rb = b_uT[seg]
                            else:
                                rhs_fn = lambda k: uTh[:, k, :]
                                rb = b_uTh
                            for k in range(16):
                                S.op("pe", lambda e: e.matmul(pa[:, 0:n], lhsT=slot[:, k, cc * 128:(cc + 1) * 128],
                                                              rhs=rhs_fn(k), start=(k == 0), stop=(k == 15)),
                                     reads=[bslot, rb], writes=[b_pa])
                            for k in range(16):
                                S.op("pe", lambda e: e.matmul(pb[:, 0:n], lhsT=slot[:, k, 256 + cc * 128:256 + (cc + 1) * 128],
                                                              rhs=rhs_fn(k), start=(k == 0), stop=(k == 15)),
                                     reads=[bslot, rb], writes=[b_pb])
                            sg, b_sg = sgt[seg % 2]
                            S.op("act", lambda e: e.activation(out=sg[:, 0:n], in_=pb[:, 0:n], func=AF.Sigmoid,
                                                               bias=V("bglu", 8 + ch)), reads=[b_pb, b_vecs], writes=[b_sg])
                            if seg < 2:
                                S.op("dve", lambda e: e.scalar_tensor_tensor(
                                    out=hT[:, ch, 30 + seg * 512:30 + (seg + 1) * 512], in0=pa[:, 0:n],
                                    scalar=V("bglu", ch), in1=sg[:, 0:n], op0=ALU.add, op1=ALU.mult),
                                    reads=[b_pa, b_sg, b_vecs], writes=[b_hT[ch]])
                            else:
                                S.op("dve", lambda e: e.scalar_tensor_tensor(
                                    out=hh[:, :], in0=pa[:, 0:n], scalar=V("bglu", ch), in1=sg[:, 0:n],
                                    op0=ALU.add, op1=ALU.mult), reads=[b_pa, b_sg, b_vecs], writes=[b_hh])
                                S.op("dve", lambda e: e.tensor_scalar(out=hT[:, ch, 0:30], in0=hh[:, 98:128],
                                                                      scalar1=V("halo"), scalar2=None, op0=ALU.mult),
                                     reads=[b_hh, b_vecs], writes=[b_hT[ch]])
                            unit += 1
                            for _ in range(2 if unit % 3 == 0 else 1):
                                mod_compute()
                                mod_issue()
                while mstate["done"] < NMB:
                    mod_compute()
                    mod_issue()
                S.op("dve", lambda e: e.tensor_tensor(out=modT[:, 32:96], in0=pmod2[:, :], in1=V("bcond", 32, 96), op=ALU.add),
                     reads=[b_pmod2, b_vecs], writes=[b_modT])
                S.op("dve", lambda e: e.tensor_scalar(out=opm[:, 32:96], in0=modT[:, 32:96], scalar1=1.0, scalar2=None,
                                                      op0=ALU.add), reads=[b_modT], writes=[b_opm])
                S.barrier()
            dump("hT", hT[:, :, :], [128, 8, 30 + NT], BF16, reads=b_hT)
            with ExitStack() as st2, ExitStack() as pst:
                wdwT = sb(st2, "wdwT", [128, 8 * CW], F32); b_wdwT = Buf()
                S.dma("sp", wdwT[:], wdwT_in[:, :], writes=[b_wdwT])
                diag = [(sb(st2, f"diag{i}", [128, CW, 128], BF16), Buf()) for i in range(2)]
                cbk = [(ps(pst, f"cbk{i}", [128, 512]), Buf()) for i in range(2)]
                mp = ps(pst, "mpc", [128, 512]); b_mp = Buf()
                ep = ps(pst, "epc", [128, 512]); b_ep = Buf()
                sqr = [(sb(st2, f"sqc{i}", [128, 512], F32), Buf()) for i in range(2)]
                msq = sb(st2, "msqc", [128, 512], F32); b_msq = Buf()
                meanb = sb(st2, "meanbc", [128, NT], F32)
                rstdb = sb(st2, "rstdbc", [128, NT], F32); b_stat = Buf()
                b_cv = [Buf() for _ in range(8)]
                cv = lambda c, half: mu_f[:, c * 1024 + half * 512:c * 1024 + (half + 1) * 512]
                ncb = 0
                diagb = [(Buf(), Buf()) for _ in range(2)]

                def build_diag(c):
                    dg, _ = diag[c % 2]
                    b_e, b_o = diagb[c % 2]
                    for k in range(CW):
                        if k % 2 == 0:
                            S.op("dve", lambda e: e.tensor_scalar(out=dg[:, k, :], in0=identf[:, :],
                                                                  scalar1=wdwT[:, c * CW + k:c * CW + k + 1],
                                                                  scalar2=None, op0=ALU.mult),
                                 reads=[b_idf, b_wdwT], writes=[b_e])
                        else:
                            S.op("act", lambda e: e.activation(out=dg[:, k, :], in_=identf[:, :], func=AF.Identity,
                                                               scale=wdwT[:, c * CW + k:c * CW + k + 1]),
                                 reads=[b_idf, b_wdwT], writes=[b_o])

                build_diag(0)
                for c in range(8):
                    dg, _ = diag[c % 2]
                    b_e, b_o = diagb[c % 2]
                    if c + 1 < 8:
                        build_diag(c + 1)
                    for half in range(2):
                        cb, b_cb = cbk[ncb % 2]; ncb += 1
                        for k in range(CW):
                            S.op("pe", lambda e: e.matmul(cb[:, :], lhsT=dg[:, k, :],
                                                          rhs=hT[:, c, half * 512 + k:half * 512 + k + 512],
                                                          start=(k == 0), stop=(k == CW - 1)),
                                 reads=[b_e, b_o, b_hT[c]], writes=[b_cb])
                        S.op("act", lambda e: e.activation(out=cv(c, half), in_=cb[:, :], func=AF.Identity,
                                                           bias=V("bdw", c)), reads=[b_cb, b_vecs], writes=[b_cv[c]])
                if dbg:
                    dump("cv", mu_f, [128, 8192], F32, reads=b_cv)
                ln_stats(cv, lambda c: b_cv[c], 8, onesC, b_onesC, meanb, rstdb, b_stat, sqr, mp, b_mp, ep, b_ep,
                         msq, b_msq)
                for c in range(8):
                    for half in range(2):
                        hs = slice(half * 512, (half + 1) * 512)
                        S.op("dve", lambda e: e.tensor_tensor(out=cv(c, half), in0=cv(c, half), in1=meanb[:, hs],
                                                              op=ALU.subtract), reads=[b_cv[c], b_stat], writes=[b_cv[c]])
                        S.op("dve", lambda e: e.tensor_tensor(out=cv(c, half), in0=cv(c, half), in1=rstdb[:, hs],
                                                              op=ALU.mult), reads=[b_cv[c], b_stat], writes=[b_cv[c]])
                        S.op("act", lambda e: e.activation(out=sT[:, c, hs], in_=cv(c, half), func=AF.Silu,
                                                           scale=V("gcn", c), bias=V("bcn", c)),
                             reads=[b_cv[c], b_vecs], writes=[b_sT])
                dump("sT", sT, [128, 8, NT], BF16, reads=[b_sT])
                S.barrier()
        if stage <= 2:
            sub1.close()
            finish()
            return nc, dbg_outs

        with ExitStack() as st, ExitStack() as pst:
            blksel = sb(st, "blksel", [16, 16 * 128], BF16); b_blksel = Buf()
            S.dma("pool", blksel[:], blksel_in[:, :], writes=[b_blksel])
            negm8 = sb(st, "negm8", [128, 8, 16], F32); b_negm = Buf()
            force8 = sb(st, "force8", [128, 8, 16], F32); b_force = Buf()
            S.dma("sp", negm8[:].rearrange("p a b -> p (a b)"), negmask_in[:, :], writes=[b_negm])
            S.dma("sp", force8[:].rearrange("p a b -> p (a b)"), force_in[:, :], writes=[b_force])
            kmb = sb(st, "kmb", [128, NH, 16], BF16); b_kmb = Buf()
            S.op("dve", lambda e: e.tensor_scalar(out=kmb[:], in0=kmean[:], scalar1=1.0 / 256.0, scalar2=None,
                                                  op0=ALU.mult), reads=[b_kmean], writes=[b_kmb])
            QTh = [(sb(st, f"QTh{i}", [128, NT], BF16), Buf()) for i in range(2)]
            Qsq = sb(st, "Qsq", [128, NT], BF16); b_Qsq = Buf()
            augTh = [(sb(st, f"augTh{i}", [16, NT], BF16), Buf()) for i in range(2)]
            Gm = sb(st, "Gm", [128, 8, 16], F32); b_g = Buf()
            m8 = sb(st, "m8", [128, 8, 8], F32)
            sel = sb(st, "sel", [128, 8, 16], F32)
            val = sb(st, "val", [128, 8, 16], F32)
            mq = sb(st, "mq", [128, 8], F32)
            rtmp = ((sb(st, "krq", [32, 512], F32), sb(st, "t1q", [32, 512], F32)), Buf())
            PTs = [(sb(st, f"PT{i}", [128, 512], BF16), Buf()) for i in range(3)]
            rD = sb(st, "rD", [128, 512], F32); b_rD = Buf()
            R = [(ps(pst, f"R{i}", [128, 512]), Buf()) for i in range(3)]
            Obs = [(ps(pst, f"Ob{i}", [128, 512]), Buf()) for i in range(2)]
            Dn = ps(pst, "Dn", [128, 512]); b_Dn = Buf()
            misc = ps(pst, "misc", [128, 512])
            gp = misc[:, 0:136].rearrange("p (a b) -> p a b", b=17); b_gp = Buf()
            tp = misc[:, 256:512]; b_tp = Buf()
            qbk = ps(pst, "qbk", [128, 512]); b_qbk = Buf()
            nR = [0]
            npt = [0]
            KV = []
            for i in range(2):
                slot = ring[i][0]
                KV.append((slot[:, 0:8, :].rearrange("p a b -> p (a b)"),
                           slot[:, 8:16, :].rearrange("p a (c d) -> p (a c) d", d=128), Buf(), Buf()))
            wq, b_wq = ring[2]

            def load_kv(h):
                kh, vh, b_kh, b_vh = KV[h % 2]
                S.dma("sp", kh, Kt_d[h, :, :], writes=[b_kh])
                S.dma("sp", vh, V_d[:, h * 128:(h + 1) * 128].rearrange("(c p) d -> p c d", p=128), writes=[b_vh])

            def part_A(h):
                if h % 4 == 0:
                    c0 = OFF_Q + (h // 4) * 512
                    S.dma("pool", wq[:, :, :], w_in[:, c0:c0 + 512].rearrange("(c p) n -> p c n", p=128), writes=[b_wq])
                qt, b_qt = QTh[h % 2]
                for half in range(2):
                    hs = slice(half * 512, (half + 1) * 512)
                    qb, b_qb = qbk, b_qbk
                    for k in range(16):
                        S.op("pe", lambda e: e.matmul(qb[:, :], lhsT=wq[:, k, (h % 4) * 128:(h % 4 + 1) * 128],
                                                      rhs=uT[:, k, hs], start=(k == 0), stop=(k == 15)),
                             reads=[b_wq, b_uT[half]], writes=[b_qb])
                    S.op("act", lambda e: e.activation(out=qt[:, hs], in_=qb[:, :], func=AF.Identity),
                         reads=[b_qb], writes=[b_qt])
                    rope_rows(qb, b_qb, cso[:, hs], sno[:, hs], qt[0:32, hs], b_qt, rtmp[0], rtmp[1], prot, b_prot,
                              qbk, b_qbk, 512, extra_reads=[b_tabo[half]])

            def part_B(h):
                qt, b_qt = QTh[h % 2]
                S.op("act", lambda e: e.activation(out=Qsq[:, :], in_=qt[:, :], func=AF.Square),
                     reads=[b_qt], writes=[b_Qsq])
                for t in range(8):
                    S.op("pe", lambda e: e.matmul(gp[:, t, 0:16], lhsT=qt[:, t * 128:(t + 1) * 128], rhs=kmb[:, h, :],
                                                  start=True, stop=True), reads=[b_qt, b_kmb], writes=[b_gp])
                    S.op("pe", lambda e: e.matmul(gp[:, t, 16:17], lhsT=Qsq[:, t * 128:(t + 1) * 128], rhs=onesb[:, 0:1],
                                                  start=True, stop=True), reads=[b_Qsq, b_onesb], writes=[b_gp])
                S.op("dve", lambda e: e.tensor_tensor(out=Gm[:], in0=gp[:, :, 0:16], in1=negm8[:], op=ALU.add),
                     reads=[b_gp, b_negm], writes=[b_g])
                for t in range(8):
                    S.op("dve", lambda e: e.max(out=m8[:, t, :], in_=Gm[:, t, :]), reads=[b_g], writes=[b_g])
                S.op("dve", lambda e: e.tensor_tensor(out=sel[:], in0=Gm[:], in1=m8[:, :, 2:3].to_broadcast([128, 8, 16]),
                                                      op=ALU.is_ge), reads=[b_g], writes=[b_g])
                S.op("dve", lambda e: e.tensor_scalar(out=val[:], in0=Gm[:], scalar1=-1e29, scalar2=None, op0=ALU.is_gt),
                     reads=[b_g], writes=[b_g])
                S.op("dve", lambda e: e.tensor_tensor(out=sel[:], in0=sel[:], in1=val[:], op=ALU.mult),
                     reads=[b_g], writes=[b_g])
                S.op("dve", lambda e: e.tensor_tensor(out=sel[:], in0=sel[:], in1=force8[:], op=ALU.max),
                     reads=[b_g, b_force], writes=[b_g])
                S.op("dve", lambda e: e.tensor_scalar(out=mq[:], in0=gp[:, :, 16], scalar1=kmax2[:, h:h + 1], scalar2=None,
                                                      op0=ALU.mult), reads=[b_gp, b_kmax2], writes=[b_g])
                S.op("act", lambda e: e.activation(out=mq[:], in_=mq[:], func=AF.Sqrt), reads=[b_g], writes=[b_g])
                S.op("dve", lambda e: e.tensor_scalar(out=sel[:], in0=sel[:], scalar1=-1.0, scalar2=MASKV,
                                                      op0=ALU.add, op1=ALU.mult), reads=[b_g], writes=[b_g])
                S.op("dve", lambda e: e.tensor_tensor(out=sel[:], in0=sel[:],
                                                      in1=mq[:].unsqueeze(2).to_broadcast([128, 8, 16]),
                                                      op=ALU.subtract), reads=[b_g], writes=[b_g])

            def part_D(h):
                aug, b_aug = augTh[h % 2]
                for rnd in range(4):
                    for i in range(2):
                        t = rnd * 2 + i
                        S.op("pe", lambda e: e.transpose(out=tp[0:16, i * 128:(i + 1) * 128], in_=sel[:, t, :],
                                                         identity=identf[:, :]), reads=[b_g, b_idf], writes=[b_tp])
                    S.op("act", lambda e: e.activation(out=aug[0:16, rnd * 256:(rnd + 1) * 256], in_=tp[0:16, :],
                                                       func=AF.Identity), reads=[b_tp], writes=[b_aug])

            load_kv(0)
            part_A(0); part_B(0); part_D(0)
            for h in range(NH):
                if h + 1 < NH:
                    load_kv(h + 1)
                qt, b_qt = QTh[h % 2]
                aug, b_aug = augTh[h % 2]
                kh, vh, b_kh, b_vh = KV[h % 2]
                gidx = 0
                for half in range(2):
                    hs = slice(half * 512, (half + 1) * 512)
                    Ob, b_Ob = Obs[half]
                    kcs = list(range(24)) + [24 + i for i in range(4 if half == 0 else 8)]
                    nck = len(kcs)
                    sc = {}

                    def emit_scores(idx):
                        kc = kcs[idx]
                        slot_i = kc // 2
                        sp_, b_sp = R[nR[0] % 3]; nR[0] += 1
                        S.op("pe", lambda e: e.matmul(sp_[:, :], lhsT=kh[:, kc * 128:(kc + 1) * 128], rhs=qt[:, hs],
                                                      start=True, stop=False), reads=[b_kh, b_qt], writes=[b_sp])
                        S.op("pe", lambda e: e.matmul(sp_[:, :], lhsT=blksel[0:16, slot_i * 128:(slot_i + 1) * 128],
                                                      rhs=aug[0:16, hs], start=False, stop=True),
                             reads=[b_blksel, b_aug], writes=[b_sp])
                        sc[idx] = (sp_, b_sp)

                    emit_scores(0)
                    emit_scores(1)
                    for idx, kc in enumerate(kcs):
                        sp_, b_sp = sc.pop(idx)
                        pt, b_pt = PTs[npt[0] % 3]; npt[0] += 1
                        S.op("act", lambda e: e.activation(out=pt[:, :], in_=sp_[:, :], func=AF.Exp, scale=float(SCALE)),
                             reads=[b_sp], writes=[b_pt])
                        if kc >= 24:
                            ko = kc - 24
                            kb_ = ko // 2
                            if kb_ // 2 == half:
                                c0 = (kb_ % 2) * 256
                                S.op("pool", lambda e: e.affine_select(out=pt[:, c0:c0 + 256], in_=pt[:, c0:c0 + 256],
                                                                       pattern=[[1, 256]], compare_op=ALU.is_ge,
                                                                       fill=0.0, base=-(ko % 2) * 128,
                                                                       channel_multiplier=-1),
                                     reads=[b_pt], writes=[b_pt])
                        if idx + 2 < nck:
                            emit_scores(idx + 2)
                        last = idx == nck - 1
                        S.op("pe", lambda e: e.matmul(Ob[:, :], lhsT=vh[:, kc, :], rhs=pt[:, :], start=(idx == 0), stop=last),
                             reads=[b_vh, b_pt], writes=[b_Ob])
                        S.op("pe", lambda e: e.matmul(Dn[:, :], lhsT=onesb[:, :], rhs=pt[:, :], start=(idx == 0), stop=last),
                             reads=[b_onesb, b_pt], writes=[b_Dn])
                        gidx += 1
                        if h + 1 < NH:
                            if gidx == 3:
                                part_A(h + 1)
                            elif gidx == 14:
                                part_B(h + 1)
                            elif gidx == 40:
                                part_D(h + 1)
                    S.op("dve", lambda e: e.tensor_scalar(out=rD[:, :], in0=Dn[:, :], scalar1=1e-30, scalar2=None,
                                                          op0=ALU.add), reads=[b_Dn], writes=[b_rD])
                    S.op("dve", lambda e: e.reciprocal(out=rD[:, :], in_=rD[:, :]), reads=[b_rD], writes=[b_rD])
                    S.op("dve", lambda e: e.tensor_tensor(out=attnT[:, h, hs], in0=Ob[:, :], in1=rD[:, :], op=ALU.mult),
                         reads=[b_Ob, b_rD], writes=[b_attnT[h]])
            dump("attnT", attnT[:, :, :], [128, NH, NT], BF16, reads=b_attnT)
            S.barrier()
        if stage <= 3:
            sub1.close()
            finish()
            return nc, dbg_outs

        with ExitStack() as st, ExitStack() as pst:
            hslots = [(ring[s_][0][:, :, hh_ * 256:(hh_ + 1) * 256], Buf()) for s_ in range(3) for hh_ in range(2)]
            mstate2 = {"issued": 0}

            def m_issue_upto(j_end):
                while mstate2["issued"] < min(24, j_end):
                    j = mstate2["issued"]
                    db_, kind = j // 3, j % 3
                    hsl, b_h = hslots[j % 6]
                    c_ = db_ * 256
                    if kind == 0:
                        S.dma("pool", hsl, w_in[:, OFF_GC + c_:OFF_GC + c_ + 256].rearrange("(c p) n -> p c n", p=128),
                              writes=[b_h])
                    elif kind == 1:
                        S.dma("pool", hsl, w_in[:, OFF_GA + c_:OFF_GA + c_ + 256].rearrange("(c p) n -> p c n", p=128),
                              writes=[b_h])
                    else:
                        S.dma("pool", hsl[:, 0:8, :], w_conv_out[:, c_:c_ + 256].rearrange("(c p) n -> p c n", p=128),
                              writes=[b_h])
                        S.dma("pool", hsl[:, 8:16, :], w_attn_out[:, c_:c_ + 256].rearrange("(c p) n -> p c n", p=128),
                              writes=[b_h])
                    mstate2["issued"] += 1

            bk = [(ps(pst, f"mb{i}", [128, 512]), Buf()) for i in range(8)]
            tmpf = [(sb(st, f"mt{i}", [128, 512], F32), Buf()) for i in range(8)]
            nbk_ = 0
            m_issue_upto(3)
            for db in range(8):
                m_issue_upto(3 * (db + 2))
                gcs, b_gcs = hslots[(3 * db) % 6]
                gas, b_gas = hslots[(3 * db + 1) % 6]
                cas, b_cas = hslots[(3 * db + 2) % 6]
                for dc in range(2):
                    dch = db * 2 + dc
                    cols = slice(dc * 128, (dc + 1) * 128)
                    for half in range(2):
                        hs = slice(half * 512, (half + 1) * 512)
                        (gcb, b_gcb), (gab, b_gab), (ycb, b_ycb), (yab, b_yab) = [bk[(nbk_ + i) % 8] for i in range(4)]
                        (sgc, b_sgc), (sga, b_sga), (t1, b_t1), (t2, b_t2) = [tmpf[(nbk_ + i) % 8] for i in range(4)]
                        nbk_ += 4
                        for k in range(16):
                            S.op("pe", lambda e: e.matmul(gcb[:, :], lhsT=gcs[:, k, cols], rhs=uT[:, k, hs],
                                                          start=(k == 0), stop=(k == 15)),
                                 reads=[b_gcs, b_uT[half]], writes=[b_gcb])
                        for k in range(16):
                            S.op("pe", lambda e: e.matmul(gab[:, :], lhsT=gas[:, k, cols], rhs=uT[:, k, hs],
                                                          start=(k == 0), stop=(k == 15)),
                                 reads=[b_gas, b_uT[half]], writes=[b_gab])
                        for k in range(8):
                            S.op("pe", lambda e: e.matmul(ycb[:, :], lhsT=cas[:, k, cols], rhs=sT[:, k, hs],
                                                          start=(k == 0), stop=(k == 7)),
                                 reads=[b_cas, b_sT], writes=[b_ycb])
                        for k in range(8):
                            S.op("pe", lambda e: e.matmul(yab[:, :], lhsT=cas[:, 8 + k, cols], rhs=attnT[:, k, hs],
                                                          start=(k == 0), stop=(k == 7)),
                                 reads=[b_cas, b_attnT[k]], writes=[b_yab])
                        S.op("act", lambda e: e.activation(out=sgc[:, :], in_=gcb[:, :], func=AF.Sigmoid),
                             reads=[b_gcb], writes=[b_sgc])
                        S.op("act", lambda e: e.activation(out=sga[:, :], in_=gab[:, :], func=AF.Sigmoid),
                             reads=[b_gab], writes=[b_sga])
                        S.op("dve", lambda e: e.scalar_tensor_tensor(out=t1[:, :], in0=ycb[:, :], scalar=V("bco", dch),
                                                                     in1=sgc[:, :], op0=ALU.add, op1=ALU.mult),
                             reads=[b_ycb, b_sgc, b_vecs], writes=[b_t1])
                        S.op("dve", lambda e: e.tensor_tensor(out=t2[:, :], in0=yab[:, :], in1=sga[:, :], op=ALU.mult),
                             reads=[b_yab, b_sga], writes=[b_t2])
                        S.op("dve", lambda e: e.tensor_tensor(out=mu[:, dch, hs], in0=t1[:, :], in1=t2[:, :], op=ALU.add),
                             reads=[b_t1, b_t2], writes=[b_mu[half]])
            dump("mergedT", mu[:, :, :], [128, 16, NT], BF16, reads=b_mu)
            S.barrier()
        sub1.close()
        if stage <= 4:
            finish()
            return nc, dbg_outs

        stB = ExitStack()
        big = sb(stB, "big", [128, 16, NT], F32); b_big = [Buf() for _ in range(16)]
        gateT = sb(stB, "gateT", [32, NT], F32); b_gateT = Buf()
        bg = lambda c, half: big[:, c, half * 512:(half + 1) * 512]
        with ExitStack() as st, ExitStack() as pst:
            xs = [(sb(st, f"xs{i}", [128, 8, 512], F32), Buf()) for i in range(1)]
            tgt = [(sb(st, f"tg{i}", [128, 512], F32), Buf()) for i in range(1)]
            tb = [(ps(pst, f"tb{i}", [128, 512]), Buf()) for i in range(2)]
            xTb = [(ps(pst, f"xTb{i}", [128, 512]), Buf()) for i in range(2)]
            mp = ps(pst, "mp1", [128, 512]); b_mp = Buf()
            ep = ps(pst, "ep1", [128, 512]); b_ep = Buf()
            lpb = ps(pst, "lpb", [128, 288]); b_lp = Buf()
            gtpb = ps(pst, "gtpb", [32, 512]); b_gtp = Buf()
            sqr = [(sb(st, f"sq1{i}", [128, 512], F32), Buf()) for i in range(2)]
            msq = sb(st, "msq1", [128, 512], F32); b_msq = Buf()
            meanb = sb(st, "meanb1", [128, NT], F32)
            rstdb = sb(st, "rstdb1", [128, NT], F32); b_stat = Buf()
            ws = WStream([colblock(w_mix_out, db * 512) for db in range(4)], nslots=4, lookahead=1)
            n2 = 0
            for db in range(4):
                slot, bslot = ws.get(db)
                xsl, b_xsl = xs[0]
                S.dma("sp", xsl[:, :, :], x_own[:, db * 512:(db + 1) * 512].rearrange("(t p) n -> p t n", p=128),
                      writes=[b_xsl])
                for dc in range(4):
                    dch = db * 4 + dc
                    for half in range(2):
                        hs = slice(half * 512, (half + 1) * 512)
                        tbk, b_tbk = tb[n2 % 2]
                        xb, b_xb = xTb[n2 % 2]
                        tg, b_tg = tgt[0]
                        n2 += 1
                        for k in range(16):
                            S.op("pe", lambda e: e.matmul(tbk[:, :], lhsT=slot[:, k, dc * 128:(dc + 1) * 128],
                                                          rhs=mu[:, k, hs], start=(k == 0), stop=(k == 15)),
                                 reads=[bslot, b_mu[half]], writes=[b_tbk])
                        for tt in range(4):
                            S.op("pe", lambda e: e.transpose(out=xb[:, tt * 128:(tt + 1) * 128],
                                                             in_=xsl[:, half * 4 + tt, dc * 128:(dc + 1) * 128],
                                                             identity=identf[:, :]),
                                 reads=[b_xsl, b_idf], writes=[b_xb])
                        S.op("act", lambda e: e.activation(out=tg[:, :], in_=tbk[:, :], func=AF.Identity,
                                                           scale=opm[:, GT1 + dch:GT1 + dch + 1]),
                             reads=[b_tbk, b_opm], writes=[b_tg])
                        S.op("dve", lambda e: e.scalar_tensor_tensor(out=bg(dch, half), in0=xb[:, :], scalar=float(ALPHA),
                                                                     in1=tg[:, :], op0=ALU.mult, op1=ALU.add),
                             reads=[b_xb, b_tg], writes=[b_big[dch]])
            ln_stats(bg, lambda c: b_big[c], 16, onesD, b_onesD, meanb, rstdb, b_stat, sqr, mp, b_mp, ep, b_ep, msq, b_msq)
            for c in range(16):
                for half in range(2):
                    hs = slice(half * 512, (half + 1) * 512)
                    S.op("dve",
                         lambda e: e.tensor_tensor(out=bg(c, half), in0=bg(c, half), in1=meanb[:, hs],
                                                   op=ALU.subtract), reads=[b_big[c], b_stat], writes=[b_big[c]])
                    S.op("dve", lambda e: e.scalar_tensor_tensor(out=bg(c, half), in0=bg(c, half), scalar=V("gln1", c),
                                                                 in1=rstdb[:, hs], op0=ALU.mult, op1=ALU.mult),
                         reads=[b_big[c], b_stat, b_vecs], writes=[b_big[c]])
                    S.op("act", lambda e: e.activation(out=bg(c, half), in_=bg(c, half), func=AF.Identity,
                                                       bias=V("bln1", c)), reads=[b_big[c], b_vecs], writes=[b_big[c]])
            b_x1d = Buf()
            S.dma("sp", x1_d, big[:].rearrange("p a b -> p (a b)"), reads=b_big, writes=[b_x1d])
            dump("x1T", big[:, :, :], [128, 16, NT], F32, reads=b_big)
            ln_stats(bg, lambda c: b_big[c], 16, onesD, b_onesD, meanb, rstdb, b_stat, sqr, mp, b_mp, ep, b_ep, msq, b_msq)
            for c in range(16):
                for half in range(2):
                    hs = slice(half * 512, (half + 1) * 512)
                    S.op("dve",
                         lambda e: e.tensor_tensor(out=bg(c, half), in0=bg(c, half), in1=meanb[:, hs],
                                                   op=ALU.subtract), reads=[b_big[c], b_stat], writes=[b_big[c]])
                    S.op("dve", lambda e: e.scalar_tensor_tensor(out=bg(c, half), in0=bg(c, half),
                                                                 scalar=opm[:, SC2 + c:SC2 + c + 1],
                                                                 in1=rstdb[:, hs], op0=ALU.mult, op1=ALU.mult),
                         reads=[b_big[c], b_stat, b_opm], writes=[b_big[c]])
                    S.op("act", lambda e: e.activation(out=bg(c, half), in_=bg(c, half), func=AF.Identity,
                                                       bias=modT[:, SH2 + c:SH2 + c + 1]),
                         reads=[b_big[c], b_modT], writes=[b_big[c]])
                    S.op("act", lambda e: e.activation(out=mu[:, c, hs], in_=bg(c, half), func=AF.Identity),
                         reads=[b_big[c]], writes=[b_mu[half]])
            dump("u2T", big[:, :, :], [128, 16, NT], F32, reads=b_big)
            wr = sb(st, "wr", [128, 16 * 36], F32); b_wr = Buf()
            brow = sb(st, "brow", [128, 36], F32); b_brow = Buf()
            S.dma("sp", wr[:], wr_in[:, :], writes=[b_wr])
            S.dma("sp", brow[:], brow_in[:, :], writes=[b_brow])
            b_r = Buf()
            lg = sb(st, "lg", [128, 8, 36], F32)
            gmx = sb(st, "gmx", [128, 8], F32)
            goh = sb(st, "goh", [128, 8, 4], F32)
            exg = sb(st, "exg", [128, 8, 4], F32)
            sume = sb(st, "sume", [128, 8], F32)
            ptop = sb(st, "ptop", [128, 8], F32)
            ig = sb(st, "ig", [128, 8, 8], F32)
            tm8 = sb(st, "tm8", [128, 8, 8], F32)
            ig8 = sb(st, "ig8", [128, 8, 8], F32)
            dlt = sb(st, "dlt", [128, 8], F32)
            w1p = sb(st, "w1p", [128, 8], F32)
            w2p = sb(st, "w2p", [128, 8], F32)
            e1 = sb(st, "e1", [128, 8, 8], F32)
            e2 = sb(st, "e2", [128, 8, 8], F32)
            g32 = sb(st, "g32", [128, 8, 32], F32)
            for t in range(8):
                ts_ = slice(t * 128, (t + 1) * 128)
                for c in range(16):
                    S.op("pe", lambda e: e.matmul(lpb[:, t * 36:(t + 1) * 36], lhsT=big[:, c, ts_], rhs=wr[:, c * 36:(c + 1) * 36],
                                                  start=(c == 0), stop=(c == 15)), reads=[b_big[c], b_wr], writes=[b_lp])
            B3 = lambda ap, n: ap.unsqueeze(2).to_broadcast([128, 8, n])
            S.op("dve", lambda e: e.tensor_tensor(out=lg[:], in0=lpb[:, 0:288].rearrange("p (a b) -> p a b", b=36),
                                                  in1=brow[:, :].unsqueeze(1).to_broadcast([128, 8, 36]), op=ALU.add),
                 reads=[b_lp, b_brow], writes=[b_r])
            S.op("dve", lambda e: e.tensor_reduce(out=gmx[:], in_=lg[:, :, 0:4], axis=AX, op=ALU.max),
                 reads=[b_r], writes=[b_r])
            S.op("dve", lambda e: e.tensor_tensor(out=goh[:], in0=lg[:, :, 0:4], in1=B3(gmx[:], 4), op=ALU.is_ge),
                 reads=[b_r], writes=[b_r])
            S.op("dve", lambda e: e.tensor_tensor(out=exg[:], in0=lg[:, :, 0:4], in1=B3(gmx[:], 4), op=ALU.subtract),
                 reads=[b_r], writes=[b_r])
            S.op("act", lambda e: e.activation(out=exg[:], in_=exg[:], func=AF.Exp), reads=[b_r], writes=[b_r])
            S.op("dve", lambda e: e.tensor_reduce(out=sume[:], in_=exg[:], axis=AX, op=ALU.add), reads=[b_r], writes=[b_r])
            S.op("dve", lambda e: e.reciprocal(out=ptop[:], in_=sume[:]), reads=[b_r], writes=[b_r])
            for g_ in range(4):
                dst_ = ig if g_ == 0 else tm8
                S.op("dve", lambda e: e.tensor_tensor(out=dst_[:], in0=lg[:, :, 4 + 8 * g_:12 + 8 * g_],
                                                      in1=goh[:, :, g_:g_ + 1].to_broadcast([128, 8, 8]), op=ALU.mult),
                     reads=[b_r], writes=[b_r])
                if g_ > 0:
                    S.op("dve", lambda e: e.tensor_tensor(out=ig[:], in0=ig[:], in1=tm8[:], op=ALU.add),
                         reads=[b_r], writes=[b_r])
            for t in range(8):
                S.op("dve", lambda e: e.max(out=ig8[:, t, :], in_=ig[:, t, :]), reads=[b_r], writes=[b_r])
            S.op("dve", lambda e: e.tensor_tensor(out=dlt[:], in0=ig8[:, :, 1], in1=ig8[:, :, 0], op=ALU.subtract),
                 reads=[b_r], writes=[b_r])
            S.op("act", lambda e: e.activation(out=dlt[:], in_=dlt[:], func=AF.Exp), reads=[b_r], writes=[b_r])
            S.op("dve", lambda e: e.tensor_scalar(out=dlt[:], in0=dlt[:], scalar1=1.0, scalar2=None, op0=ALU.add),
                 reads=[b_r], writes=[b_r])
            S.op("dve", lambda e: e.reciprocal(out=w1p[:], in_=dlt[:]), reads=[b_r], writes=[b_r])
            S.op("dve", lambda e: e.tensor_tensor(out=w1p[:], in0=w1p[:], in1=ptop[:], op=ALU.mult), reads=[b_r], writes=[b_r])
            S.op("dve", lambda e: e.tensor_tensor(out=w2p[:], in0=ptop[:], in1=w1p[:], op=ALU.subtract),
                 reads=[b_r], writes=[b_r])
            S.op("dve", lambda e: e.tensor_tensor(out=e1[:], in0=ig[:], in1=ig8[:, :, 0:1].to_broadcast([128, 8, 8]),
                                                  op=ALU.is_equal), reads=[b_r], writes=[b_r])
            S.op("dve", lambda e: e.tensor_tensor(out=e1[:], in0=e1[:], in1=B3(w1p[:], 8), op=ALU.mult),
                 reads=[b_r], writes=[b_r])
            S.op("dve", lambda e: e.tensor_tensor(out=e2[:], in0=ig[:], in1=ig8[:, :, 1:2].to_broadcast([128, 8, 8]),
                                                  op=ALU.is_equal), reads=[b_r], writes=[b_r])
            S.op("dve", lambda e: e.tensor_tensor(out=e2[:], in0=e2[:], in1=B3(w2p[:], 8), op=ALU.mult),
                 reads=[b_r], writes=[b_r])
            S.op("dve", lambda e: e.tensor_tensor(out=e1[:], in0=e1[:], in1=e2[:], op=ALU.add), reads=[b_r], writes=[b_r])
            for g_ in range(4):
                S.op("dve", lambda e: e.tensor_tensor(out=g32[:, :, 8 * g_:8 * g_ + 8], in0=e1[:],
                                                      in1=goh[:, :, g_:g_ + 1].to_broadcast([128, 8, 8]), op=ALU.mult),
                     reads=[b_r], writes=[b_r])
            for rnd in range(2):
                for i in range(4):
                    t = rnd * 4 + i
                    S.op("pe", lambda e: e.transpose(out=gtpb[0:32, i * 128:(i + 1) * 128], in_=g32[:, t, :],
                                                     identity=identf[:, :]), reads=[b_r, b_idf], writes=[b_gtp])
                S.op("act", lambda e: e.activation(out=gateT[0:32, rnd * 512:(rnd + 1) * 512], in_=gtpb[0:32, :],
                                                   func=AF.Identity), reads=[b_gtp], writes=[b_gateT])
            b_gated = Buf()
            S.dma("sp", gate_d, gateT[:, :], reads=[b_gateT], writes=[b_gated])
            dump("lg", lg[:, :, :], [128, 8, 36], F32, reads=[b_r])
            dump("gateT", gateT[:, :], [32, NT], F32, reads=[b_gateT])
            S.barrier()
        if stage <= 5:
            stB.close()
            finish()
            return nc, dbg_outs

        with ExitStack() as st, ExitStack() as pst:
            gTs = [(sb(st, f"gT{i}", [128, 4, NT], BF16), Buf()) for i in range(2)]
            gbs = [(sb(st, f"gb{i}", [128, NT], F32), Buf()) for i in range(2)]
            s1s = [(sb(st, f"s1{i}", [128, 512], F32), Buf()) for i in range(2)]
            ggs = [(sb(st, f"gg{i}", [128, 512], F32), Buf()) for i in range(2)]
            hb = [(ps(pst, f"hb{i}", [128, 512]), Buf()) for i in range(4)]
            yb = [(ps(pst, f"yb{i}", [128, 512]), Buf()) for i in range(3)]
            blocks = []
            for e_ in range(NEXP):
                blocks.append(colblock(w1[e_], 0))
                blocks.append(colblock(w3[e_], 0))
                blocks.append([(lambda s: s[:].rearrange("p (f a) b -> p f (a b)", f=4),
                                w2[e_].rearrange("(f p) d -> p f d", p=128))])
            ws = WStream(blocks, nslots=4, lookahead=1)
            nh = 0
            ny = 0
            for e_ in range(NEXP):
                w1s, b_w1s = ws.get(3 * e_)
                w3s, b_w3s = ws.get(3 * e_ + 1)
                w2s_, b_w2s = ws.get(3 * e_ + 2)
                w2s = w2s_[:].rearrange("p (f a) b -> p f (a b)", f=4)
                gb, b_gb = gbs[e_ % 2]
                S.dma("sp", gb[:, :], gate_d[e_:e_ + 1, :].to_broadcast([128, NT]), writes=[b_gb])
                gT, b_gT = gTs[e_ % 2]
                for fc in range(4):
                    for half in range(2):
                        hs = slice(half * 512, (half + 1) * 512)
                        h1b, b_h1b = hb[nh % 4]; nh += 1
                        h3b, b_h3b = hb[nh % 4]; nh += 1
                        for k in range(16):
                            S.op("pe", lambda e: e.matmul(h1b[:, :], lhsT=w1s[:, k, fc * 128:(fc + 1) * 128], rhs=mu[:, k, hs],
                                                          start=(k == 0), stop=(k == 15)),
                                 reads=[b_w1s, b_mu[half]], writes=[b_h1b])
                        for k in range(16):
                            S.op("pe", lambda e: e.matmul(h3b[:, :], lhsT=w3s[:, k, fc * 128:(fc + 1) * 128], rhs=mu[:, k, hs],
                                                          start=(k == 0), stop=(k == 15)),
                                 reads=[b_w3s, b_mu[half]], writes=[b_h3b])
                        s1, b_s1 = s1s[(nh // 2) % 2]
                        gg, b_gg = ggs[(nh // 2) % 2]
                        S.op("act", lambda e: e.activation(out=s1[:, :], in_=h1b[:, :], func=AF.Silu),
                             reads=[b_h1b], writes=[b_s1])
                        S.op("dve", lambda e: e.tensor_tensor(out=gg[:, :], in0=s1[:, :], in1=h3b[:, :], op=ALU.mult),
                             reads=[b_s1, b_h3b], writes=[b_gg])
                        S.op("dve", lambda e: e.tensor_tensor(out=gT[:, fc, hs], in0=gg[:, :], in1=gb[:, hs], op=ALU.mult),
                             reads=[b_gg, b_gb], writes=[b_gT])
                for dc in range(16):
                    for half in range(2):
                        hs = slice(half * 512, (half + 1) * 512)
                        ybk, b_ybk = yb[ny % 3]; ny += 1
                        for fc in range(4):
                            S.op("pe", lambda e: e.matmul(ybk[:, :], lhsT=w2s[:, fc, dc * 128:(dc + 1) * 128], rhs=gT[:, fc, hs],
                                                          start=(fc == 0), stop=(fc == 3)),
                                 reads=[b_w2s, b_gT], writes=[b_ybk])
                        if e_ == 0:
                            S.op("act", lambda e: e.activation(out=bg(dc, half), in_=ybk[:, :], func=AF.Identity),
                                 reads=[b_ybk], writes=[b_big[dc]])
                        else:
                            S.op("dve", lambda e: e.tensor_tensor(out=bg(dc, half), in0=bg(dc, half), in1=ybk[:, :],
                                                                  op=ALU.add), reads=[b_ybk, b_big[dc]], writes=[b_big[dc]])
            dump("fT", big[:, :, :], [128, 16, NT], F32, reads=b_big)
            S.barrier()

        with ExitStack() as st, ExitStack() as pst:
            x1c = [(sb(st, f"x1c{i}", [128, 512], F32), Buf()) for i in range(4)]
            mp = ps(pst, "mp2", [128, 512]); b_mp = Buf()
            ep = ps(pst, "ep2", [128, 512]); b_ep = Buf()
            ob = [(ps(pst, f"ob{i}", [128, 512]), Buf()) for i in range(3)]
            sqr = [(sb(st, f"sq2{i}", [128, 512], F32), Buf()) for i in range(2)]
            msq = sb(st, "msq2", [128, 512], F32); b_msq = Buf()
            meanb = sb(st, "meanb2", [128, NT], F32)
            rstdb = sb(st, "rstdb2", [128, NT], F32); b_stat = Buf()
            otile = [(sb(st, f"ot{i}", [128, D], F32), Buf()) for i in range(2)]
            n3 = 0
            for c in range(16):
                for half in range(2):
                    xc, b_xc = x1c[n3 % 4]; n3 += 1
                    S.dma("sp", xc[:, :], x1_d[:, c * NT + half * 512:c * NT + (half + 1) * 512], reads=[b_x1d], writes=[b_xc])
                    S.op("act", lambda e: e.activation(out=xc[:, :], in_=xc[:, :], func=AF.Identity, scale=float(ALPHA)),
                         reads=[b_xc], writes=[b_xc])
                    S.op("dve", lambda e: e.scalar_tensor_tensor(out=bg(c, half), in0=bg(c, half),
                                                                 scalar=opm[:, GT2 + c:GT2 + c + 1], in1=xc[:, :],
                                                                 op0=ALU.mult, op1=ALU.add),
                         reads=[b_big[c], b_xc, b_opm], writes=[b_big[c]])
            ln_stats(bg, lambda c: b_big[c], 16, onesD, b_onesD, meanb, rstdb, b_stat, sqr, mp, b_mp, ep, b_ep, msq, b_msq)
            for c in range(16):
                for half in range(2):
                    hs = slice(half * 512, (half + 1) * 512)
                    S.op("dve",
                         lambda e: e.tensor_tensor(out=bg(c, half), in0=bg(c, half), in1=meanb[:, hs],
                                                   op=ALU.subtract), reads=[b_big[c], b_stat], writes=[b_big[c]])
                    S.op("dve", lambda e: e.scalar_tensor_tensor(out=bg(c, half), in0=bg(c, half), scalar=V("gln2", c),
                                                                 in1=rstdb[:, hs], op0=ALU.mult, op1=ALU.mult),
                         reads=[b_big[c], b_stat, b_vecs], writes=[b_big[c]])
                    S.op("act", lambda e: e.activation(out=bg(c, half), in_=bg(c, half), func=AF.Identity,
                                                       bias=V("bln2", c)), reads=[b_big[c], b_vecs], writes=[b_big[c]])
            no = 0
            for t in range(8):
                ot, b_ot = otile[t % 2]
                for cg in range(4):
                    obk, b_obk = ob[no % 3]; no += 1
                    for i in range(4):
                        c = cg * 4 + i
                        S.op("pe", lambda e: e.transpose(out=obk[:, i * 128:(i + 1) * 128], in_=big[:, c, t * 128:(t + 1) * 128],
                                                         identity=identf[:, :]), reads=[b_big[c], b_idf], writes=[b_obk])
                    if cg % 2 == 0:
                        S.op("act", lambda e: e.activation(out=ot[:, cg * 512:(cg + 1) * 512], in_=obk[:, :], func=AF.Identity),
                             reads=[b_obk], writes=[b_ot])
                    else:
                        S.op("dve", lambda e: e.tensor_copy(out=ot[:, cg * 512:(cg + 1) * 512], in_=obk[:, :]),
                             reads=[b_obk], writes=[b_ot])
                S.dma("sp", out[t * 128:(t + 1) * 128, :], ot[:, :], reads=[b_ot], writes=[Buf()])
        stB.close()
        finish()
    return nc, dbg_outs


def _fm(v, nch):
    return np.ascontiguousarray(np.asarray(v, np.float32).reshape(nch, 128).T)


def make_in_maps(inputs):
    x = np.asarray(inputs["x"], np.float32)
    c = np.asarray(inputs["c"], np.float32)
    positions = np.asarray(inputs["positions"], np.int32)
    g = lambda n: np.asarray(inputs[n])[0]
    half = 8
    invf = (500000.0 ** (-np.arange(half, dtype=np.float32) / np.float32(half))).astype(np.float32)
    invf16 = np.zeros(16, np.float32)
    invf16 = np.power(np.float32(500000.0), -np.arange(16, dtype=np.float32) / np.float32(16)).astype(np.float32)
    prot = np.zeros((32, 32), np.float32)
    for m in range(16):
        prot[m + 16, m] = -1.0
        prot[m, m + 16] = 1.0
    blksel = np.zeros((16, 16, 128), np.float32)
    for s in range(16):
        blksel[s, s, :] = 1.0
    blksel = blksel.reshape(16, 16 * 128)
    wdw = g("w_dw")
    wdwT = np.ascontiguousarray(wdw.reshape(CW, 8, 128).transpose(2, 1, 0).reshape(128, 8 * CW)).astype(np.float32)
    wr_full = np.concatenate([g("w_grp"), g("w_erouter")], axis=1)
    wr = np.ascontiguousarray(wr_full.reshape(16, 128, 36).transpose(1, 0, 2).reshape(128, 16 * 36)).astype(np.float32)
    brow = np.tile(np.concatenate([g("b_grp"), g("b_erouter")])[None, :], (128, 1)).astype(np.float32)
    shared = {
        "wdwT": wdwT, "brow": brow, "wr": wr, "prot": prot, "blksel": blksel,
        "w_cond": np.ascontiguousarray(g("w_cond")), "w_in": np.ascontiguousarray(g("w_in")),
        "w_conv_out": np.ascontiguousarray(g("w_conv_out")), "w_attn_out": np.ascontiguousarray(g("w_attn_out")),
        "w_mix_out": np.ascontiguousarray(g("w_mix_out")),
        "w1": np.ascontiguousarray(g("w1").reshape(NEXP, D, FF)),
        "w3": np.ascontiguousarray(g("w3").reshape(NEXP, D, FF)),
        "w2": np.ascontiguousarray(g("w2").reshape(NEXP, FF, D)),
    }
    maps = []
    for core in range(8):
        b, j = core // 4, core % 4
        s0 = j * NT
        vec = np.zeros((128, NV), np.float32)

        def put(name, arr):
            o, w = VOFF[name]
            vec[:, o:o + w] = arr
        put("bglu", _fm(g("b_glu"), 16)); put("bdw", _fm(g("b_dw"), 8)); put("gcn", _fm(g("g_cn"), 8))
        put("bcn", _fm(g("b_cn"), 8)); put("bco", _fm(g("b_conv_out"), 16)); put("gln1", _fm(g("g_ln1"), 16))
        put("bln1", _fm(g("b_ln1"), 16)); put("gln2", _fm(g("g_ln2"), 16)); put("bln2", _fm(g("b_ln2"), 16))
        put("bcond", _fm(g("b_cond"), 96)); put("c", _fm(c[b], 16))
        put("halo", np.full((128, 1), 1.0 if j > 0 else 0.0, np.float32))
        iv = np.zeros((128, 1), np.float32)
        iv[0:32, 0] = np.concatenate([invf16, invf16])
        put("invf", iv)
        if j > 0:
            x_halo = x[b, s0 - 128:s0]
        else:
            x_halo = np.zeros((128, D), np.float32)
        pos = np.concatenate([positions[b, 0:NCTX], positions[b, s0:s0 + NT]]).astype(np.int32)
        negmask = np.zeros((8, 16), np.float32)
        force = np.zeros((8, 16), np.float32)
        for tq in range(8):
            lb = tq // 2
            for s in range(12):
                negmask[tq, s] = 0.0 if s < 4 * j else -1e30
            for i in range(4):
                negmask[tq, 12 + i] = 0.0 if i < lb else -1e30
            force[tq, 12 + lb] = 1.0
        m = dict(shared)
        m.update({
            "x_own": np.ascontiguousarray(x[b, s0:s0 + NT]),
            "x_halo": np.ascontiguousarray(x_halo),
            "x_ctx": np.ascontiguousarray(x[b, 0:NCTX]),
            "pos": np.ascontiguousarray(np.tile(pos[None, :], (32, 1))),
            "vecs": vec,
            "negmask": np.tile(negmask.reshape(1, 128), (128, 1)).astype(np.float32),
            "force": np.tile(force.reshape(1, 128), (128, 1)).astype(np.float32),
        })
        maps.append(m)
    return maps


_CACHE = {}


def kernel(**inputs):
    if "nc" not in _CACHE:
        _CACHE["nc"] = build_program()[0]
    nc = _CACHE["nc"]
    maps = make_in_maps(inputs)
    res = run_bass_kernel_spmd(nc, maps, core_ids=list(range(8)))
    outp = np.zeros((2, SEQ, D), np.float32)
    for core in range(8):
        b, j = core // 4, core % 4
        outp[b, j * NT:(j + 1) * NT] = res.results[core]["out"]
    return outp
```

```python
import numpy as np
from contextlib import ExitStack
import concourse.bass as bass
import concourse.mybir as mybir
from concourse.bass_utils import run_bass_kernel_spmd

F32 = mybir.dt.float32
BF16 = mybir.dt.bfloat16
I32 = mybir.dt.int32
AF = mybir.ActivationFunctionType
ALU = mybir.AluOpType

D = 2048
SEQ = 4096
NT = 1024
NCTX = 3072
NKEY = NCTX + NT
CONV_CH = 1024
CW = 31
NH = 8
HD = 128
IN_COLS = 9216
OFF_Q = 2048
OFF_K = 3072
OFF_V = 4096
OFF_GC = 5120
OFF_GA = 7168
NEXP = 32
FF = 512
EPS = 1e-5
ALPHA = 2.0 ** 0.25
SCALE = HD ** -0.5
TWO_PI = 2.0 * np.pi
MASKV = 3.0e4

VOFF = {}
_o = 0
for _n, _w in [("bglu", 16), ("bdw", 8), ("gcn", 8), ("bcn", 8), ("bco", 16), ("gln1", 16),
               ("bln1", 16), ("gln2", 16), ("bln2", 16), ("bcond", 96), ("c", 16), ("halo", 1),
               ("invf", 1)]:
    VOFF[_n] = (_o, _w)
    _o += _w
NV = _o


class Buf:
    __slots__ = ("w", "r")

    def __init__(self):
        self.w = None
        self.r = {}


class Sched:
    def __init__(self, nc, stack, nlanes=6):
        self.nc = nc
        self.E = {}
        for name, h in [("pe", nc.tensor), ("act", nc.scalar), ("dve", nc.vector),
                        ("pool", nc.gpsimd), ("sp", nc.sync)]:
            sem = stack.enter_context(nc.semaphore("s_" + name))
            self.E[name] = dict(h=h, sem=sem, cnt=0, waited={}, name=name)
        self.lanes = {}
        for q in ("sp", "pool"):
            self.lanes[q] = [dict(sem=stack.enter_context(nc.semaphore(f"l_{q}{i}")), cnt=0)
                             for i in range(nlanes)]
        self.rr = {q: 0 for q in self.lanes}
        self.nwaits = 0
        self.nops = 0

    def _wait(self, eng, ev):
        sem, val = ev
        e = self.E[eng]
        if sem is e["sem"] and eng == "pe":
            return
        k = id(sem)
        if e["waited"].get(k, 0) >= val:
            return
        e["h"].wait_ge(sem, val)
        e["waited"][k] = val
        self.nwaits += 1

    def _deps(self, eng, reads, writes):
        for b in reads:
            if b.w is not None:
                self._wait(eng, b.w)
        for b in writes:
            if b.w is not None:
                self._wait(eng, b.w)
            for ev in b.r.values():
                self._wait(eng, ev)

    def _commit(self, ev, reads, writes):
        k = id(ev[0])
        for b in reads:
            old = b.r.get(k)
            if old is None or old[1] < ev[1]:
                b.r[k] = ev
        for b in writes:
            b.w = ev
            b.r = {}

    def op(self, eng, fn, reads=(), writes=()):
        e = self.E[eng]
        self._deps(eng, reads, writes)
        ins = fn(e["h"])
        e["cnt"] += 1
        ins.then_inc(e["sem"], 1)
        ev = (e["sem"], e["cnt"])
        self._commit(ev, reads, writes)
        self.nops += 1
        return ev

    def dma(self, q, out, in_, reads=(), writes=()):
        e = self.E[q]
        lanes = self.lanes[q]
        ln = lanes[self.rr[q] % len(lanes)]
        self.rr[q] += 1
        self._deps(q, reads, writes)
        if ln["cnt"] > 0:
            self._wait(q, (ln["sem"], 16 * ln["cnt"]))
        ins = e["h"].dma_start(out=out, in_=in_)
        ln["cnt"] += 1
        ins.then_inc(ln["sem"], 16)
        ev = (ln["sem"], 16 * ln["cnt"])
        self._commit(ev, reads, writes)
        self.nops += 1
        return ev

    def all_events(self):
        evs = []
        for e in self.E.values():
            if e["cnt"]:
                evs.append((e["sem"], e["cnt"]))
        for lanes in self.lanes.values():
            for ln in lanes:
                if ln["cnt"]:
                    evs.append((ln["sem"], 16 * ln["cnt"]))
        return evs

    def barrier(self, engines=("pe", "act", "dve", "pool", "sp")):
        evs = self.all_events()
        for eng in engines:
            for ev in evs:
                self._wait(eng, ev)


def build_program(stage=99, dbg=False):
    nc = bass.Bass("TRN2", target_bir_lowering=False)

    def din(name, shape, dt=F32):
        return nc.dram_tensor(name, list(shape), dt, kind="ExternalInput").ap()

    x_own = din("x_own", [NT, D])
    x_halo = din("x_halo", [128, D])
    x_ctx = din("x_ctx", [NCTX, D])
    pos_in = din("pos", [32, NKEY], I32)
    vecs_in = din("vecs", [128, NV])
    negmask_in = din("negmask", [128, 128])
    force_in = din("force", [128, 128])
    wdwT_in = din("wdwT", [128, 8 * CW])
    brow_in = din("brow", [128, 36])
    wr_in = din("wr", [128, 16 * 36])
    prot_in = din("prot", [32, 32])
    blksel_in = din("blksel", [16, 16 * 128])
    w_cond = din("w_cond", [D, 6 * D])
    w_in = din("w_in", [D, IN_COLS])
    w_conv_out = din("w_conv_out", [CONV_CH, D])
    w_attn_out = din("w_attn_out", [NH * HD, D])
    w_mix_out = din("w_mix_out", [D, D])
    w1 = din("w1", [NEXP, D, FF])
    w3 = din("w3", [NEXP, D, FF])
    w2 = din("w2", [NEXP, FF, D])
    out = nc.dram_tensor("out", [NT, D], F32, kind="ExternalOutput").ap()

    Kt_d = nc.dram_tensor("Kt_d", [NH, 128, NKEY], BF16, kind="Internal").ap()
    V_d = nc.dram_tensor("V_d", [NKEY, NH * HD], BF16, kind="Internal").ap()
    x1_d = nc.dram_tensor("x1_d", [128, 16 * NT], F32, kind="Internal").ap()
    gate_d = nc.dram_tensor("gate_d", [NEXP, NT], F32, kind="Internal").ap()

    dbg_outs = {}

    with ExitStack() as top:
        S = Sched(nc, top)

        uid = [0]

        def sb(st, name, shape, dt):
            uid[0] += 1
            return st.enter_context(nc.sbuf_tensor(f"s{uid[0]}_{name}", list(shape), dt))

        def ps(st, name, shape, dt=F32):
            uid[0] += 1
            return st.enter_context(nc.psum_tensor(f"p{uid[0]}_{name}", list(shape), dt))

        def dump(name, src_ap, shape, dt=F32, reads=()):
            if not dbg:
                return
            t = nc.dram_tensor("dbg_" + name, list(shape), dt, kind="ExternalOutput").ap()
            S.dma("sp", t, src_ap, reads=list(reads), writes=[Buf()])
            dbg_outs[name] = (list(shape), dt)

        def finish():
            S.barrier(engines=("sp",))

        vecs = sb(top, "vecs", [128, NV], F32); b_vecs = Buf()
        identf = sb(top, "identf", [128, 128], F32); b_idf = Buf()
        identb = sb(top, "identb", [128, 128], BF16); b_idb = Buf()
        onesD = sb(top, "onesD", [128, 128], F32); b_onesD = Buf()
        onesC = sb(top, "onesC", [128, 128], F32); b_onesC = Buf()
        onesb = sb(top, "onesb", [128, 128], BF16); b_onesb = Buf()
        modT = sb(top, "modT", [128, 96], F32); b_modT = Buf()
        opm = sb(top, "opm", [128, 96], F32); b_opm = Buf()
        ring = [(sb(top, f"ring{i}", [128, 16, 512], BF16), Buf()) for i in range(4)]
        ring_pos = [0]

        def V(name, c0=0, c1=None):
            o, w = VOFF[name]
            if c1 is None:
                c1 = c0 + 1
            return vecs[:, o + c0:o + c1]

        S.dma("sp", vecs[:], vecs_in[:, :], writes=[b_vecs])
        S.op("pool", lambda e: e.memset(identf[:], 0.0), writes=[b_idf])
        S.op("pool", lambda e: e.affine_select(out=identf[:], in_=identf[:], pattern=[[-1, 128]],
                                               compare_op=ALU.not_equal, fill=1.0, base=0,
                                               channel_multiplier=1), reads=[b_idf], writes=[b_idf])
        S.op("dve", lambda e: e.tensor_copy(out=identb[:], in_=identf[:]), reads=[b_idf], writes=[b_idb])
        S.op("dve", lambda e: e.memset(onesD[:], 1.0 / D), writes=[b_onesD])
        S.op("dve", lambda e: e.memset(onesC[:], 1.0 / CONV_CH), writes=[b_onesC])
        S.op("dve", lambda e: e.memset(onesb[:], 1.0), writes=[b_onesb])

        class WStream:
            def __init__(self, blocks, nslots=4, lookahead=2):
                self.blocks = blocks
                self.issued = 0
                self.la = lookahead
                self.nslots = nslots
                self.slots = {}

            def _issue(self, i):
                slot, b = ring[i % self.nslots]
                for fn, src in self.blocks[i]:
                    S.dma("pool", fn(slot), src, writes=[b])
                self.slots[i] = (slot, b)

            def get(self, i):
                while self.issued < min(len(self.blocks), i + 1 + self.la):
                    self._issue(self.issued)
                    self.issued += 1
                return self.slots[i]

        def colblock(w_ap, c0, ncols=512, kc=16):
            return [(lambda s: s[:, 0:kc, 0:ncols],
                     w_ap[:, c0:c0 + ncols].rearrange("(c p) n -> p c n", p=128))]

        csb = sb(top, "csb", [128, 16], BF16); b_csb = Buf()
        S.op("act", lambda e: e.activation(out=csb[:], in_=V("c", 0, 16), func=AF.Silu), reads=[b_vecs], writes=[b_csb])
        with ExitStack() as st:
            pmod = ps(st, "pmod", [128, 32]); b_pmod = Buf()
            ws = WStream([colblock(w_cond, i * 512) for i in range(8)], nslots=4, lookahead=2)
            for i in range(8):
                slot, bslot = ws.get(i)
                for cc in range(4):
                    q = i * 4 + cc
                    for k in range(16):
                        S.op("pe", lambda e: e.matmul(pmod[:, q:q + 1], lhsT=slot[:, k, cc * 128:(cc + 1) * 128],
                                                      rhs=csb[:, k:k + 1], start=(k == 0), stop=(k == 15)),
                             reads=[bslot, b_csb], writes=[b_pmod])
            S.op("dve", lambda e: e.tensor_tensor(out=modT[:, 0:32], in0=pmod[:, 0:32], in1=V("bcond", 0, 32), op=ALU.add),
                 reads=[b_pmod, b_vecs], writes=[b_modT])
            S.op("dve", lambda e: e.tensor_scalar(out=opm[:, 0:32], in0=modT[:, 0:32], scalar1=1.0, scalar2=None, op0=ALU.add),
                 reads=[b_modT], writes=[b_opm])
            S.barrier()
        if stage <= 0:
            finish()
            return nc, dbg_outs

        SH1, SC1, GT1, SH2, SC2, GT2 = 0, 16, 32, 48, 64, 80

        def ln_tile_to_uT(st_tag, xt, b_xt, work, dst_fn, b_dst, pT, b_pT):
            stats, mv, rstd, nmr, xn, b_w = work
            for s4 in range(4):
                S.op("dve", lambda e: e.bn_stats(out=stats[:, s4, :], in_=xt[:, s4 * 512:(s4 + 1) * 512]),
                     reads=[b_xt], writes=[b_w])
            S.op("dve", lambda e: e.bn_aggr(out=mv[:], in_=stats[:].rearrange("p a b -> p (a b)")),
                 reads=[b_w], writes=[b_w])
            S.op("dve", lambda e: e.tensor_scalar(out=rstd[:], in0=mv[:, 1:2], scalar1=EPS, scalar2=None,
                                                  op0=ALU.add), reads=[b_w], writes=[b_w])
            S.op("act", lambda e: e.activation(out=rstd[:], in_=rstd[:], func=AF.Sqrt), reads=[b_w], writes=[b_w])
            S.op("dve", lambda e: e.reciprocal(out=rstd[:], in_=rstd[:]), reads=[b_w], writes=[b_w])
            S.op("dve", lambda e: e.scalar_tensor_tensor(out=nmr[:], in0=mv[:, 0:1], scalar=-1.0, in1=rstd[:],
                                                         op0=ALU.mult, op1=ALU.mult), reads=[b_w], writes=[b_w])
            S.op("act", lambda e: e.activation(out=xn[:], in_=xt[:], func=AF.Identity, bias=nmr[:], scale=rstd[:]),
                 reads=[b_xt, b_w], writes=[b_w])
            for c in range(16):
                S.op("pe", lambda e: e.transpose(out=pT[:, c * 128:(c + 1) * 128], in_=xn[:, c * 128:(c + 1) * 128],
                                                 identity=identb[:]), reads=[b_w, b_idb], writes=[b_pT])
            for c in range(16):
                if c % 2 == 0:
                    S.op("dve", lambda e: e.tensor_scalar(out=dst_fn(c), in0=pT[:, c * 128:(c + 1) * 128],
                                                          scalar1=opm[:, SC1 + c:SC1 + c + 1],
                                                          scalar2=modT[:, SH1 + c:SH1 + c + 1],
                                                          op0=ALU.mult, op1=ALU.add),
                         reads=[b_pT, b_opm, b_modT], writes=[b_dst])
                else:
                    S.op("act", lambda e: e.activation(out=dst_fn(c), in_=pT[:, c * 128:(c + 1) * 128],
                                                       func=AF.Identity, scale=opm[:, SC1 + c:SC1 + c + 1],
                                                       bias=modT[:, SH1 + c:SH1 + c + 1]),
                         reads=[b_pT, b_opm, b_modT], writes=[b_dst])

        def rope_rows(src_ps, b_src, cs_ap, sn_ap, dst_ap, b_dst, tmp, b_tmp, prot, b_prot, prot_ps, b_prps, n, extra_reads=()):
            kr, t1 = tmp
            S.op("act", lambda e: e.activation(out=kr[0:32, 0:n], in_=src_ps[0:32, 0:n], func=AF.Identity),
                 reads=[b_src], writes=[b_tmp])
            S.op("pe", lambda e: e.matmul(prot_ps[0:32, 0:n], lhsT=prot[0:32, 0:32], rhs=kr[0:32, 0:n],
                                          start=True, stop=True), reads=[b_tmp, b_prot], writes=[b_prps])
            S.op("dve", lambda e: e.tensor_tensor(out=t1[0:32, 0:n], in0=kr[0:32, 0:n], in1=cs_ap, op=ALU.mult),
                 reads=[b_tmp] + list(extra_reads), writes=[b_tmp])
            S.op("dve", lambda e: e.tensor_tensor(out=kr[0:32, 0:n], in0=prot_ps[0:32, 0:n], in1=sn_ap, op=ALU.mult),
                 reads=[b_prps, b_tmp] + list(extra_reads), writes=[b_tmp])
            S.op("dve", lambda e: e.tensor_tensor(out=dst_ap, in0=kr[0:32, 0:n], in1=t1[0:32, 0:n], op=ALU.add),
                 reads=[b_tmp], writes=[b_dst])

        AX = mybir.AxisListType.X
        mu = sb(top, "mu", [128, 16, NT], BF16)
        b_mu = [Buf(), Buf()]
        mu_f = mu[:].bitcast(F32).rearrange("p a b -> p (a b)")
        sT = ring[3][0][:].rearrange("p a b -> p (a b)").rearrange("p (c t) -> p c t", c=8)
        b_sT = Buf()
        sub1 = ExitStack()
        uT = sb(sub1, "uT", [128, 16, NT], BF16)
        b_uT = [Buf(), Buf()]
        cso = sb(sub1, "cso", [32, NT], F32)
        sno = sb(sub1, "sno", [32, NT], F32)
        b_tabo = [Buf(), Buf()]
        kmean = sb(sub1, "kmean", [128, NH, 16], F32); b_kmean = Buf()
        kmax2 = sb(sub1, "kmax2", [128, NH], F32); b_kmax2 = Buf()
        prot = sb(sub1, "prot", [32, 32], F32); b_prot = Buf()
        attnT = sb(sub1, "attnT", [128, NH, NT], BF16); b_attnT = [Buf() for _ in range(NH)]
        S.dma("sp", prot[:], prot_in[:, :], writes=[b_prot])

        def rope_tables(col0, n, cs_ap, sn_ap, b_tab, tmps, b_tmp, deferred=None, ops_out=None):
            posi, yv, fv_s, ki, mk, fv_c = tmps
            ops = []

            def D(fn):
                ops.append(lambda: S.op("dve", fn, reads=[b_tmp, b_vecs], writes=[b_tmp]))

            ops.append(lambda: S.dma("sp", posi[:, 0:n], pos_in[:, col0:col0 + n], writes=[b_tmp]))
            D(lambda e: e.tensor_copy(out=yv[:, 0:n], in_=posi[:, 0:n]))
            D(lambda e: e.tensor_scalar(out=yv[:, 0:n], in0=yv[:, 0:n], scalar1=V("invf")[0:32, :],
                                        scalar2=float(1.0 / TWO_PI), op0=ALU.mult, op1=ALU.mult))
            sins = []
            for which, dst, fv in (("sin", sn_ap, fv_s), ("cos", cs_ap, fv_c)):
                if which == "cos":
                    D(lambda e: e.tensor_scalar(out=yv[:, 0:n], in0=yv[:, 0:n], scalar1=0.25, scalar2=None, op0=ALU.add))
                D(lambda e, fv=fv: e.tensor_copy(out=ki[:, 0:n], in_=yv[:, 0:n]))
                D(lambda e, fv=fv: e.tensor_copy(out=fv[:, 0:n], in_=ki[:, 0:n]))
                D(lambda e, fv=fv: e.tensor_tensor(out=fv[:, 0:n], in0=yv[:, 0:n], in1=fv[:, 0:n], op=ALU.subtract))
                D(lambda e, fv=fv: e.tensor_scalar(out=mk[:, 0:n], in0=fv[:, 0:n], scalar1=0.5, scalar2=None, op0=ALU.is_gt))
                D(lambda e, fv=fv: e.tensor_tensor(out=fv[:, 0:n], in0=fv[:, 0:n], in1=mk[:, 0:n], op=ALU.subtract))
                D(lambda e, fv=fv: e.tensor_scalar(out=mk[:, 0:n], in0=fv[:, 0:n], scalar1=-0.5, scalar2=None, op0=ALU.is_lt))
                D(lambda e, fv=fv: e.tensor_tensor(out=fv[:, 0:n], in0=fv[:, 0:n], in1=mk[:, 0:n], op=ALU.add))

                def sin_op(dst=dst, fv=fv):
                    S.op("act", lambda e: e.activation(out=dst, in_=fv[:, 0:n], func=AF.Sin, scale=float(TWO_PI)),
                         reads=[b_tmp], writes=[b_tab])
                sins.append(sin_op)
            if ops_out is None:
                for o in ops:
                    o()
            else:
                ops_out.extend(ops)
            if deferred is None:
                for f_ in sins:
                    f_()
            else:
                deferred.extend(sins)

        def ln_stats(src, b_src, C, ones, b_ones, meanb, rstdb, b_stat, sqr, mp, b_mp, ep, b_ep, msq, b_msq):
            for half in range(2):
                hs = slice(half * 512, (half + 1) * 512)
                for c in range(C):
                    sq, b_sq = sqr[c % 2]
                    S.op("act", lambda e: e.activation(out=sq[:, :], in_=src(c, half), func=AF.Square),
                         reads=[b_src(c)], writes=[b_sq])
                    S.op("pe", lambda e: e.matmul(mp[:, :], lhsT=ones[:, :], rhs=src(c, half), start=(c == 0),
                                                  stop=(c == C - 1)), reads=[b_src(c), b_ones], writes=[b_mp])
                    S.op("pe", lambda e: e.matmul(ep[:, :], lhsT=ones[:, :], rhs=sq[:, :], start=(c == 0),
                                                  stop=(c == C - 1)), reads=[b_sq, b_ones], writes=[b_ep])
                S.op("act", lambda e: e.activation(out=meanb[:, hs], in_=mp[:, :], func=AF.Identity),
                     reads=[b_mp], writes=[b_stat])
                S.op("act", lambda e: e.activation(out=msq[:, :], in_=mp[:, :], func=AF.Square),
                     reads=[b_mp], writes=[b_msq])
                S.op("dve", lambda e: e.tensor_tensor(out=rstdb[:, hs], in0=ep[:, :], in1=msq[:, :], op=ALU.subtract),
                     reads=[b_ep, b_msq], writes=[b_stat])
                S.op("dve", lambda e: e.tensor_scalar(out=rstdb[:, hs], in0=rstdb[:, hs], scalar1=0.0, scalar2=EPS,
                                                      op0=ALU.max, op1=ALU.add), reads=[b_stat], writes=[b_stat])
                S.op("act", lambda e: e.activation(out=rstdb[:, hs], in_=rstdb[:, hs], func=AF.Sqrt),
                     reads=[b_stat], writes=[b_stat])
                S.op("dve", lambda e: e.reciprocal(out=rstdb[:, hs], in_=rstdb[:, hs]), reads=[b_stat], writes=[b_stat])

        with ExitStack() as st, ExitStack() as pst:
            wk0, bwk0 = ring[0]
            wk1, bwk1 = ring[1]
            wv0, bwv0 = ring[2]
            wv1, bwv1 = ring[3]
            for (slot, bslot), c0 in ((ring[0], OFF_K), (ring[1], OFF_K + 512), (ring[2], OFF_V), (ring[3], OFF_V + 512)):
                S.dma("pool", slot[:, :, :], w_in[:, c0:c0 + 512].rearrange("(c p) n -> p c n", p=128), writes=[bslot])
            xts = [(sb(st, f"xt{i}", [128, D], F32), Buf()) for i in range(2)]
            works = []
            for i in range(4):
                works.append((sb(st, f"stats{i}", [128, 4, 6], F32), sb(st, f"mv{i}", [128, 2], F32),
                              sb(st, f"rstd{i}", [128, 1], F32), sb(st, f"nmr{i}", [128, 1], F32),
                              sb(st, f"xn{i}", [128, D], BF16), Buf()))
            pT, b_pT = ps(pst, "pT", [128, D], BF16), Buf()
            banks = [(ps(pst, f"kvb{i}", [128, 512]), Buf()) for i in range(4)]
            prps = (ps(pst, "prps", [32, 512]), Buf())
            nrmb = (ps(pst, "nrmb", [128, 512]), Buf())
            ktsb = [(sb(st, f"ktsb{i}", [128, 512], BF16), Buf()) for i in range(3)]
            ksq = [(sb(st, f"ksq{i}", [128, 512], BF16), Buf()) for i in range(1)]
            vsb = [(sb(st, f"vsb{i}", [128, 1024], BF16), Buf()) for i in range(2)]
            ascr = attnT[:].bitcast(F32)
            rtmp = [((ascr[0:32, 2 * i, :], ascr[0:32, 2 * i + 1, :]), Buf()) for i in range(2)]
            tabs = [((ascr[0:32, 4 + 2 * i, :], ascr[0:32, 5 + 2 * i, :]), Buf()) for i in range(2)]
            pk = sb(st, "posi", [32, 512], I32)
            ttmp = (pk, sb(st, "yv", [32, 512], F32), sb(st, "fv", [32, 512], F32), pk, sb(st, "mk", [32, 512], F32),
                    sb(st, "fvc", [32, 512], F32))
            b_ttmp = Buf()
            kred = sb(st, "kred", [128, 2], F32); b_kred = Buf()
            S.op("dve", lambda e: e.memset(kmax2[:], 0.0), writes=[b_kmax2])
            nb = [0]

            def grp(g):
                own = g >= 6
                if own:
                    ug_buf = b_uT[g - 6]
                    ug = lambda c, t0, n: uT[:, c, (g - 6) * 512 + t0:(g - 6) * 512 + t0 + n]
                    cs_g, sn_g, b_tab = cso[:, (g - 6) * 512:(g - 5) * 512], sno[:, (g - 6) * 512:(g - 5) * 512], b_tabo[g - 6]
                else:
                    ug_buf = b_mu[g % 2]
                    ug = lambda c, t0, n: mu[:, c, (g % 2) * 512 + t0:(g % 2) * 512 + t0 + n]
                    (cs_t, sn_t), b_tab = tabs[g % 2]
                    cs_g, sn_g = cs_t, sn_t
                return own, ug, ug_buf, cs_g, sn_g, b_tab

            def ln_pre(g, t):
                own = g >= 6
                idx = g * 4 + t
                xt, b_xt = xts[idx % 2]
                stats, mv, rstd, nmr, xn, b_w = works[idx % 4]
                if own:
                    src = x_own[(g - 6) * 512 + t * 128:(g - 6) * 512 + (t + 1) * 128, :]
                else:
                    src = x_ctx[g * 512 + t * 128:g * 512 + (t + 1) * 128, :]
                S.dma("sp", xt[:], src, writes=[b_xt])
                for s4 in range(4):
                    S.op("dve", lambda e: e.bn_stats(out=stats[:, s4, :], in_=xt[:, s4 * 512:(s4 + 1) * 512]),
                         reads=[b_xt], writes=[b_w])
                S.op("dve", lambda e: e.bn_aggr(out=mv[:], in_=stats[:].rearrange("p a b -> p (a b)")),
                     reads=[b_w], writes=[b_w])
                S.op("dve", lambda e: e.tensor_scalar(out=rstd[:], in0=mv[:, 1:2], scalar1=EPS, scalar2=None,
                                                      op0=ALU.add), reads=[b_w], writes=[b_w])

            def ln_pre_b(g, t):
                idx = g * 4 + t
                xt, b_xt = xts[idx % 2]
                stats, mv, rstd, nmr, xn, b_w = works[idx % 4]
                S.op("act", lambda e: e.activation(out=rstd[:], in_=rstd[:], func=AF.Sqrt), reads=[b_w], writes=[b_w])
                S.op("dve", lambda e: e.reciprocal(out=rstd[:], in_=rstd[:]), reads=[b_w], writes=[b_w])
                S.op("dve", lambda e: e.scalar_tensor_tensor(out=nmr[:], in0=mv[:, 0:1], scalar=-1.0, in1=rstd[:],
                                                             op0=ALU.mult, op1=ALU.mult), reads=[b_w], writes=[b_w])
                S.op("act", lambda e: e.activation(out=xn[:], in_=xt[:], func=AF.Identity, bias=nmr[:], scale=rstd[:]),
                     reads=[b_xt, b_w], writes=[b_w])

            def ln_pe(g, t):
                own, ug, ug_buf, _, _, _ = grp(g)
                idx = g * 4 + t
                stats, mv, rstd, nmr, xn, b_w = works[idx % 4]
                for c in range(16):
                    S.op("pe", lambda e: e.transpose(out=pT[:, c * 128:(c + 1) * 128], in_=xn[:, c * 128:(c + 1) * 128],
                                                     identity=identb[:]), reads=[b_w, b_idb], writes=[b_pT])
                for c in range(16):
                    dst = ug(c, t * 128, 128)
                    if c % 2 == 0:
                        S.op("dve", lambda e: e.tensor_scalar(out=dst, in0=pT[:, c * 128:(c + 1) * 128],
                                                              scalar1=opm[:, SC1 + c:SC1 + c + 1],
                                                              scalar2=modT[:, SH1 + c:SH1 + c + 1],
                                                              op0=ALU.mult, op1=ALU.add),
                             reads=[b_pT, b_opm, b_modT], writes=[ug_buf])
                    else:
                        S.op("act", lambda e: e.activation(out=dst, in_=pT[:, c * 128:(c + 1) * 128],
                                                           func=AF.Identity, scale=opm[:, SC1 + c:SC1 + c + 1],
                                                           bias=modT[:, SH1 + c:SH1 + c + 1]),
                             reads=[b_pT, b_opm, b_modT], writes=[ug_buf])

            def stage_A(g, h, kb, b_kb):
                own, ug, ug_buf, cs_g, sn_g, b_tab = grp(g)
                hh = g * NH + h
                kt, b_kt = ktsb[hh % 3]
                (kr, t1), b_tmp = rtmp[hh % 2]
                prp, b_prp = prps
                S.op("pe", lambda e: e.matmul(prp[0:32, :], lhsT=prot[0:32, 0:32], rhs=kr, start=True, stop=True),
                     reads=[b_tmp, b_prot], writes=[b_prp])
                S.op("pool", lambda e: e.tensor_tensor(out=t1, in0=kr, in1=cs_g, op=ALU.mult),
                     reads=[b_tmp, b_tab], writes=[b_tmp])
                S.op("dve", lambda e: e.tensor_tensor(out=kr, in0=prp[0:32, :], in1=sn_g, op=ALU.mult),
                     reads=[b_prp, b_tmp, b_tab], writes=[b_tmp])
                S.op("pool", lambda e: e.tensor_tensor(out=kt[0:32, :], in0=kr, in1=t1, op=ALU.add),
                     reads=[b_tmp], writes=[b_kt])
                S.dma("sp", Kt_d[h, :, g * 512:(g + 1) * 512], kt[:, :], reads=[b_kt], writes=[Buf()])
                S.op("dve", lambda e: e.tensor_reduce(out=kmean[:, h, 2 * g:2 * g + 2],
                                                      in_=kt[:, :].rearrange("p (b t) -> p b t", b=2),
                                                      axis=AX, op=ALU.add), reads=[b_kt], writes=[b_kmean])
                sq, b_sq = ksq[0]
                S.op("act", lambda e: e.activation(out=sq[:, :], in_=kt[:, :], func=AF.Square),
                     reads=[b_kt], writes=[b_sq])

            def stage_B(g, h):
                hh = g * NH + h
                sq, b_sq = ksq[0]
                nbk, b_nbk = nrmb
                S.op("pe", lambda e: e.matmul(nbk[:, :], lhsT=onesb[:, :], rhs=sq[:, :], start=True, stop=True),
                     reads=[b_sq, b_onesb], writes=[b_nbk])
                S.op("dve", lambda e: e.tensor_reduce(out=kred[:, 0:1], in_=nbk[:, :], axis=AX, op=ALU.max),
                     reads=[b_nbk], writes=[b_kred])
                S.op("dve", lambda e: e.tensor_tensor(out=kmax2[:, h:h + 1], in0=kmax2[:, h:h + 1],
                                                      in1=kred[:, 0:1], op=ALU.max),
                     reads=[b_kred, b_kmax2], writes=[b_kmax2])

            pendA = []
            pendB = []

            def pump():
                if pendB:
                    stage_B(*pendB.pop(0))
                if pendA:
                    a_ = pendA.pop(0)
                    stage_A(*a_)
                    pendB.append((a_[0], a_[1]))

            own0, _, _, cs0, sn0, bt0 = grp(0)
            rope_tables(0, 512, cs0, sn0, bt0, ttmp, b_ttmp)
            for t in range(4):
                ln_pre(0, t)
                ln_pre_b(0, t)
                ln_pe(0, t)
            for g in range(8):
                own, ug, ug_buf, cs_g, sn_g, b_tab = grp(g)
                sin_def = []
                rope_ops = []
                if g + 1 < 8:
                    _, _, _, cs_n, sn_n, bt_n = grp(g + 1)
                    rope_tables((g + 1) * 512, 512, cs_n, sn_n, bt_n, ttmp, b_ttmp, deferred=sin_def, ops_out=rope_ops)
                for h in range(NH):
                    hh = g * NH + h
                    wslot, bws = (wk0, bwk0) if h < 4 else (wk1, bwk1)
                    kb, b_kb = banks[nb[0] % 4]; nb[0] += 1
                    for k in range(16):
                        S.op("pe", lambda e: e.matmul(kb[:, :], lhsT=wslot[:, k, (h % 4) * 128:(h % 4 + 1) * 128],
                                                      rhs=ug(k, 0, 512), start=(k == 0), stop=(k == 15)),
                             reads=[bws, ug_buf], writes=[b_kb])
                    kt, b_kt = ktsb[hh % 3]
                    (kr, t1), b_tmp = rtmp[hh % 2]
                    S.op("act", lambda e: e.activation(out=kt[:, :], in_=kb[:, :], func=AF.Identity),
                         reads=[b_kb], writes=[b_kt])
                    S.op("act", lambda e: e.activation(out=kr, in_=kb[0:32, :], func=AF.Identity),
                         reads=[b_kb], writes=[b_tmp])
                    pump()
                    pendA.append((g, h, kb, b_kb))
                    for _ in range(4):
                        if rope_ops:
                            rope_ops.pop(0)()
                    if h == 7:
                        for f_ in sin_def:
                            f_()
                    if g + 1 < 8:
                        if 1 <= h <= 4:
                            ln_pre_b(g + 1, h - 1)
                        if h < 4:
                            ln_pre(g + 1, h)
                        else:
                            ln_pe(g + 1, h - 4)
                for t in range(4):
                    vt, b_vt = vsb[t % 2]
                    for cb in range(2):
                        wslot, bws = (wv0, bwv0) if cb == 0 else (wv1, bwv1)
                        vb, b_vb = banks[nb[0] % 4]; nb[0] += 1
                        for k in range(16):
                            S.op("pe", lambda e: e.matmul(vb[:, :], lhsT=ug(k, t * 128, 128), rhs=wslot[:, k, :],
                                                          start=(k == 0), stop=(k == 15)),
                                 reads=[bws, ug_buf], writes=[b_vb])
                        if cb == 0:
                            S.op("act", lambda e: e.activation(out=vt[:, 0:512], in_=vb[:, :], func=AF.Identity),
                                 reads=[b_vb], writes=[b_vt])
                        else:
                            S.op("dve", lambda e: e.tensor_copy(out=vt[:, 512:1024], in_=vb[:, :]),
                                 reads=[b_vb], writes=[b_vt])
                    S.dma("sp", V_d[g * 512 + t * 128:g * 512 + (t + 1) * 128, :], vt[:, :], reads=[b_vt],
                          writes=[Buf()])
                    if t < 2:
                        pump()
            while pendA or pendB:
                pump()
            S.barrier()
        if stage <= 1:
            sub1.close()
            finish()
            return nc, dbg_outs

        with ExitStack() as st:
            hT = sb(st, "hT", [128, 8, 30 + NT], BF16); b_hT = [Buf() for _ in range(8)]
            with ExitStack() as st2, ExitStack() as pst:
                uTh = sb(st2, "uTh", [128, 16, 128], BF16); b_uTh = Buf()
                xt = sb(st2, "xth", [128, D], F32); b_xt = Buf()
                work = (sb(st2, "statsh", [128, 4, 6], F32), sb(st2, "mvh", [128, 2], F32),
                        sb(st2, "rstdh", [128, 1], F32), sb(st2, "nmrh", [128, 1], F32),
                        sb(st2, "xnh", [128, D], BF16), Buf())
                pT = ps(pst, "pTh", [128, D], BF16); b_pT = Buf()
                pab = [(ps(pst, f"pab{i}", [128, 512]), Buf()) for i in range(4)]
                sgt = [(sb(st2, f"sgt{i}", [128, 512], F32), Buf()) for i in range(2)]
                hh = sb(st2, "hh", [128, 128], F32); b_hh = Buf()
                S.dma("sp", xt[:], x_halo[:, :], writes=[b_xt])
                ln_tile_to_uT("halo", xt, b_xt, work, lambda c: uTh[:, c, :], b_uTh, pT, b_pT)
                blocks = []
                for i in range(4):
                    blocks.append([
                        (lambda s: s[:, :, 0:256], w_in[:, 256 * i:256 * i + 256].rearrange("(c p) n -> p c n", p=128)),
                        (lambda s: s[:, :, 256:512],
                         w_in[:, 1024 + 256 * i:1024 + 256 * i + 256].rearrange("(c p) n -> p c n", p=128))])
                ws = WStream(blocks, nslots=3)
                npb = 0
                pmod2 = ps(pst, "pmod2", [128, 64]); b_pmod2 = Buf()
                r3 = ring[3][0]
                wcs = [(r3[:, :, 0:256], Buf()), (r3[:, :, 256:512], Buf())]
                mstate = {"issued": 0, "done": 0}
                NMB = 16

                def mod_issue():
                    bi = mstate["issued"]
                    if bi >= NMB:
                        return
                    wt, b_wt = wcs[bi % 2]
                    c0 = 4096 + bi * 256
                    S.dma("pool", wt, w_cond[:, c0:c0 + 256].rearrange("(c p) n -> p c n", p=128), writes=[b_wt])
                    mstate["issued"] += 1

                def mod_compute():
                    bi = mstate["done"]
                    if bi >= mstate["issued"]:
                        return
                    wt, b_wt = wcs[bi % 2]
                    for c2 in range(2):
                        q = 2 * bi + c2
                        for k in range(16):
                            S.op("pe", lambda e: e.matmul(pmod2[:, q:q + 1], lhsT=wt[:, k, c2 * 128:(c2 + 1) * 128],
                                                          rhs=csb[:, k:k + 1], start=(k == 0), stop=(k == 15)),
                                 reads=[b_wt, b_csb], writes=[b_pmod2])
                    mstate["done"] += 1

                mod_issue()
                mod_issue()
                unit = 0
                for i in range(4):
                    slot, bslot = ws.get(i)
                    for cc in range(2):
                        ch = 2 * i + cc
                        for seg in range(3):
                            n = 512 if seg < 2 else 128
                            pa, b_pa = pab[npb % 4]; npb += 1
                            pb, b_pb = pab[npb % 4]; npb += 1
                            if seg < 2:
                                rhs_fn = lambda k: uT[:, k, seg * 512:(seg + 1) * 512]
                                rb = b_uT[seg]
                            else:
                                rhs_fn = lambda k: uTh[:, k, :]
                                rb = b_uTh
                            for k in range(16):
                                S.op("pe", lambda e: e.matmul(pa[:, 0:n], lhsT=slot[:, k, cc * 128:(cc + 1) * 128],
                                                              rhs=rhs_fn(k), start=(k == 0), stop=(k == 15)),
                                     reads=[bslot, rb], writes=[b_pa])
                            for k in range(16):
                                S.op("pe", lambda e: e.matmul(pb[:, 0:n], lhsT=slot[:, k, 256 + cc * 128:256 + (cc + 1) * 128],
                                                              rhs=rhs_fn(k), start=(k == 0), stop=(k == 15)),
                                     reads=[bslot, rb], writes=[b_pb])
                            sg, b_sg = sgt[seg % 2]
                            S.op("act", lambda e: e.activation(out=sg[:, 0:n], in_=pb[:, 0:n], func=AF.Sigmoid,
                                                               bias=V("bglu", 8 + ch)), reads=[b_pb, b_vecs], writes=[b_sg])
                            if seg < 2:
                                S.op("dve", lambda e: e.scalar_tensor_tensor(
                                    out=hT[:, ch, 30 + seg * 512:30 + (seg + 1) * 512], in0=pa[:, 0:n],
                                    scalar=V("bglu", ch), in1=sg[:, 0:n], op0=ALU.add, op1=ALU.mult),
                                    reads=[b_pa, b_sg, b_vecs], writes=[b_hT[ch]])
                            else:
                                S.op("dve", lambda e: e.scalar_tensor_tensor(
                                    out=hh[:, :], in0=pa[:, 0:n], scalar=V("bglu", ch), in1=sg[:, 0:n],
                                    op0=ALU.add, op1=ALU.mult), reads=[b_pa, b_sg, b_vecs], writes=[b_hh])
                                S.op("dve", lambda e: e.tensor_scalar(out=hT[:, ch, 0:30], in0=hh[:, 98:128],
                                                                      scalar1=V("halo"), scalar2=None, op0=ALU.mult),
                                     reads=[b_hh, b_vecs], writes=[b_hT[ch]])
                            unit += 1
                            mod_compute()
                            mod_issue()
                while mstate["done"] < NMB:
                    mod_compute()
                    mod_issue()
                S.op("dve", lambda e: e.tensor_tensor(out=modT[:, 32:64], in0=pmod2[:, 0:32], in1=V("bcond", 32, 64), op=ALU.add),
                     reads=[b_pmod2, b_vecs], writes=[b_modT])
                S.op("dve", lambda e: e.tensor_scalar(out=opm[:, 32:64], in0=modT[:, 32:64], scalar1=1.0, scalar2=None,
                                                      op0=ALU.add), reads=[b_modT], writes=[b_opm])
                S.barrier()
            dump("hT", hT[:, :, :], [128, 8, 30 + NT], BF16, reads=b_hT)
            with ExitStack() as st2, ExitStack() as pst:
                wdwT = sb(st2, "wdwT", [128, 8 * CW], F32); b_wdwT = Buf()
                S.dma("sp", wdwT[:], wdwT_in[:, :], writes=[b_wdwT])
                diag = [(sb(st2, f"diag{i}", [128, CW, 128], BF16), Buf()) for i in range(2)]
                cbk = [(ps(pst, f"cbk{i}", [128, 512]), Buf()) for i in range(2)]
                mp = ps(pst, "mpc", [128, 512]); b_mp = Buf()
                ep = ps(pst, "epc", [128, 512]); b_ep = Buf()
                sqr = [(sb(st2, f"sqc{i}", [128, 512], F32), Buf()) for i in range(2)]
                msq = sb(st2, "msqc", [128, 512], F32); b_msq = Buf()
                meanb = sb(st2, "meanbc", [128, NT], F32)
                rstdb = sb(st2, "rstdbc", [128, NT], F32); b_stat = Buf()
                b_cv = [Buf() for _ in range(8)]
                cv = lambda c, half: mu_f[:, c * 1024 + half * 512:c * 1024 + (half + 1) * 512]
                ncb = 0
                diagb = [(Buf(), Buf()) for _ in range(2)]

                def build_diag(c):
                    dg, _ = diag[c % 2]
                    b_e, b_o = diagb[c % 2]
                    for k in range(CW):
                        if k % 2 == 0:
                            S.op("dve", lambda e: e.tensor_scalar(out=dg[:, k, :], in0=identf[:, :],
                                                                  scalar1=wdwT[:, c * CW + k:c * CW + k + 1],
                                                                  scalar2=None, op0=ALU.mult),
                                 reads=[b_idf, b_wdwT], writes=[b_e])
                        else:
                            S.op("act", lambda e: e.activation(out=dg[:, k, :], in_=identf[:, :], func=AF.Identity,
                                                               scale=wdwT[:, c * CW + k:c * CW + k + 1]),
                                 reads=[b_idf, b_wdwT], writes=[b_o])

                pmod3 = ps(pst, "pmod3", [128, 32]); b_pmod3 = Buf()
                wcs3 = [(ring[s_][0][:, :, hh_ * 256:(hh_ + 1) * 256], Buf()) for s_ in range(2) for hh_ in range(2)]
                m3 = {"issued": 0, "done": 0}

                def mod3_issue():
                    bi = m3["issued"]
                    if bi >= 16:
                        return
                    wt, b_wt = wcs3[bi % 4]
                    c0 = 4096 + (16 + bi) * 256
                    S.dma("pool", wt, w_cond[:, c0:c0 + 256].rearrange("(c p) n -> p c n", p=128), writes=[b_wt])
                    m3["issued"] += 1

                def mod3_compute():
                    bi = m3["done"]
                    if bi >= m3["issued"]:
                        return
                    wt, b_wt = wcs3[bi % 4]
                    for c2 in range(2):
                        q = 2 * bi + c2
                        for k in range(16):
                            S.op("pe", lambda e: e.matmul(pmod3[:, q:q + 1], lhsT=wt[:, k, c2 * 128:(c2 + 1) * 128],
                                                          rhs=csb[:, k:k + 1], start=(k == 0), stop=(k == 15)),
                                 reads=[b_wt, b_csb], writes=[b_pmod3])
                    m3["done"] += 1

                mod3_issue(); mod3_issue(); mod3_issue()
                build_diag(0)
                for c in range(8):
                    dg, _ = diag[c % 2]
                    b_e, b_o = diagb[c % 2]
                    if c + 1 < 8:
                        build_diag(c + 1)
                    for half in range(2):
                        mod3_compute()
                        mod3_issue()
                        cb, b_cb = cbk[ncb % 2]; ncb += 1
                        for k in range(CW):
                            S.op("pe", lambda e: e.matmul(cb[:, :], lhsT=dg[:, k, :],
                                                          rhs=hT[:, c, half * 512 + k:half * 512 + k + 512],
                                                          start=(k == 0), stop=(k == CW - 1)),
                                 reads=[b_e, b_o, b_hT[c]], writes=[b_cb])
                        S.op("act", lambda e: e.activation(out=cv(c, half), in_=cb[:, :], func=AF.Identity,
                                                           bias=V("bdw", c)), reads=[b_cb, b_vecs], writes=[b_cv[c]])
                while m3["done"] < 16:
                    mod3_compute()
                    mod3_issue()
                S.op("dve", lambda e: e.tensor_tensor(out=modT[:, 64:96], in0=pmod3[:, 0:32], in1=V("bcond", 64, 96), op=ALU.add),
                     reads=[b_pmod3, b_vecs], writes=[b_modT])
                S.op("dve", lambda e: e.tensor_scalar(out=opm[:, 64:96], in0=modT[:, 64:96], scalar1=1.0, scalar2=None,
                                                      op0=ALU.add), reads=[b_modT], writes=[b_opm])
                if dbg:
                    dump("cv", mu_f, [128, 8192], F32, reads=b_cv)
                ln_stats(cv, lambda c: b_cv[c], 8, onesC, b_onesC, meanb, rstdb, b_stat, sqr, mp, b_mp, ep, b_ep,
                         msq, b_msq)
                for c in range(8):
                    for half in range(2):
                        hs = slice(half * 512, (half + 1) * 512)
                        S.op("dve", lambda e: e.tensor_tensor(out=cv(c, half), in0=cv(c, half), in1=meanb[:, hs],
                                                              op=ALU.subtract), reads=[b_cv[c], b_stat], writes=[b_cv[c]])
                        S.op("dve", lambda e: e.tensor_tensor(out=cv(c, half), in0=cv(c, half), in1=rstdb[:, hs],
                                                              op=ALU.mult), reads=[b_cv[c], b_stat], writes=[b_cv[c]])
                        S.op("act", lambda e: e.activation(out=sT[:, c, hs], in_=cv(c, half), func=AF.Silu,
                                                           scale=V("gcn", c), bias=V("bcn", c)),
                             reads=[b_cv[c], b_vecs], writes=[b_sT])
                dump("sT", sT, [128, 8, NT], BF16, reads=[b_sT])
                S.barrier()
        if stage <= 2:
            sub1.close()
            finish()
            return nc, dbg_outs

        with ExitStack() as st, ExitStack() as pst:
            blksel = sb(st, "blksel", [16, 16 * 128], BF16); b_blksel = Buf()
            S.dma("pool", blksel[:], blksel_in[:, :], writes=[b_blksel])
            negm8 = sb(st, "negm8", [128, 8, 16], F32); b_negm = Buf()
            force8 = sb(st, "force8", [128, 8, 16], F32); b_force = Buf()
            S.dma("sp", negm8[:].rearrange("p a b -> p (a b)"), negmask_in[:, :], writes=[b_negm])
            S.dma("sp", force8[:].rearrange("p a b -> p (a b)"), force_in[:, :], writes=[b_force])
            kmb = sb(st, "kmb", [128, NH, 16], BF16); b_kmb = Buf()
            S.op("dve", lambda e: e.tensor_scalar(out=kmb[:], in0=kmean[:], scalar1=1.0 / 256.0, scalar2=None,
                                                  op0=ALU.mult), reads=[b_kmean], writes=[b_kmb])
            QTh = [(sb(st, f"QTh{i}", [128, NT], BF16), Buf()) for i in range(2)]
            Qsq = sb(st, "Qsq", [128, NT], BF16); b_Qsq = Buf()
            augTh = [(sb(st, f"augTh{i}", [16, NT], BF16), Buf()) for i in range(2)]
            Gm = sb(st, "Gm", [128, 8, 16], F32); b_g = Buf()
            m8 = sb(st, "m8", [128, 8, 8], F32)
            sel = sb(st, "sel", [128, 8, 16], F32)
            val = sb(st, "val", [128, 8, 16], F32)
            mq = sb(st, "mq", [128, 8], F32)
            rtmp = ((sb(st, "krq", [32, 512], F32), sb(st, "t1q", [32, 512], F32)), Buf())
            PTs = [(sb(st, f"PT{i}", [128, 512], BF16), Buf()) for i in range(3)]
            rD = sb(st, "rD", [128, 512], F32); b_rD = Buf()
            R = [(ps(pst, f"R{i}", [128, 512]), Buf()) for i in range(3)]
            Obs = [(ps(pst, f"Ob{i}", [128, 512]), Buf()) for i in range(2)]
            Dn = ps(pst, "Dn", [128, 512]); b_Dn = Buf()
            misc = ps(pst, "misc", [128, 512])
            gp = misc[:, 0:136].rearrange("p (a b) -> p a b", b=17); b_gp = Buf()
            tp = misc[:, 256:512]; b_tp = Buf()
            qbk = ps(pst, "qbk", [128, 512]); b_qbk = Buf()
            nR = [0]
            npt = [0]
            KV = []
            for i in range(2):
                slot = ring[i][0]
                KV.append((slot[:, 0:8, :].rearrange("p a b -> p (a b)"),
                           slot[:, 8:16, :].rearrange("p a (c d) -> p (a c) d", d=128), Buf(), Buf()))
            wq, b_wq = ring[2]

            def load_kv(h):
                kh, vh, b_kh, b_vh = KV[h % 2]
                S.dma("sp", kh, Kt_d[h, :, :], writes=[b_kh])
                S.dma("sp", vh, V_d[:, h * 128:(h + 1) * 128].rearrange("(c p) d -> p c d", p=128), writes=[b_vh])

            def part_A(h):
                if h % 4 == 0:
                    c0 = OFF_Q + (h // 4) * 512
                    S.dma("pool", wq[:, :, :], w_in[:, c0:c0 + 512].rearrange("(c p) n -> p c n", p=128), writes=[b_wq])
                qt, b_qt = QTh[h % 2]
                for half in range(2):
                    hs = slice(half * 512, (half + 1) * 512)
                    qb, b_qb = qbk, b_qbk
                    for k in range(16):
                        S.op("pe", lambda e: e.matmul(qb[:, :], lhsT=wq[:, k, (h % 4) * 128:(h % 4 + 1) * 128],
                                                      rhs=uT[:, k, hs], start=(k == 0), stop=(k == 15)),
                             reads=[b_wq, b_uT[half]], writes=[b_qb])
                    S.op("act", lambda e: e.activation(out=qt[:, hs], in_=qb[:, :], func=AF.Identity),
                         reads=[b_qb], writes=[b_qt])
                    rope_rows(qb, b_qb, cso[:, hs], sno[:, hs], qt[0:32, hs], b_qt, rtmp[0], rtmp[1], prot, b_prot,
                              qbk, b_qbk, 512, extra_reads=[b_tabo[half]])

            def part_B(h):
                qt, b_qt = QTh[h % 2]
                S.op("act", lambda e: e.activation(out=Qsq[:, :], in_=qt[:, :], func=AF.Square),
                     reads=[b_qt], writes=[b_Qsq])
                for t in range(8):
                    S.op("pe", lambda e: e.matmul(gp[:, t, 0:16], lhsT=qt[:, t * 128:(t + 1) * 128], rhs=kmb[:, h, :],
                                                  start=True, stop=True), reads=[b_qt, b_kmb], writes=[b_gp])
                    S.op("pe", lambda e: e.matmul(gp[:, t, 16:17], lhsT=Qsq[:, t * 128:(t + 1) * 128], rhs=onesb[:, 0:1],
                                                  start=True, stop=True), reads=[b_Qsq, b_onesb], writes=[b_gp])
                S.op("dve", lambda e: e.tensor_tensor(out=Gm[:], in0=gp[:, :, 0:16], in1=negm8[:], op=ALU.add),
                     reads=[b_gp, b_negm], writes=[b_g])
                for t in range(8):
                    S.op("dve", lambda e: e.max(out=m8[:, t, :], in_=Gm[:, t, :]), reads=[b_g], writes=[b_g])
                S.op("dve", lambda e: e.tensor_tensor(out=sel[:], in0=Gm[:], in1=m8[:, :, 2:3].to_broadcast([128, 8, 16]),
                                                      op=ALU.is_ge), reads=[b_g], writes=[b_g])
                S.op("dve", lambda e: e.tensor_scalar(out=val[:], in0=Gm[:], scalar1=-1e29, scalar2=None, op0=ALU.is_gt),
                     reads=[b_g], writes=[b_g])
                S.op("dve", lambda e: e.tensor_tensor(out=sel[:], in0=sel[:], in1=val[:], op=ALU.mult),
                     reads=[b_g], writes=[b_g])
                S.op("dve", lambda e: e.tensor_tensor(out=sel[:], in0=sel[:], in1=force8[:], op=ALU.max),
                     reads=[b_g, b_force], writes=[b_g])
                S.op("dve", lambda e: e.tensor_scalar(out=mq[:], in0=gp[:, :, 16], scalar1=kmax2[:, h:h + 1], scalar2=None,
                                                      op0=ALU.mult), reads=[b_gp, b_kmax2], writes=[b_g])
                S.op("act", lambda e: e.activation(out=mq[:], in_=mq[:], func=AF.Sqrt), reads=[b_g], writes=[b_g])
                S.op("dve", lambda e: e.tensor_scalar(out=sel[:], in0=sel[:], scalar1=-1.0, scalar2=MASKV,
                                                      op0=ALU.add, op1=ALU.mult), reads=[b_g], writes=[b_g])
                S.op("dve", lambda e: e.tensor_tensor(out=sel[:], in0=sel[:],
                                                      in1=mq[:].unsqueeze(2).to_broadcast([128, 8, 16]),
                                                      op=ALU.subtract), reads=[b_g], writes=[b_g])

            def part_D(h):
                aug, b_aug = augTh[h % 2]
                for rnd in range(4):
                    for i in range(2):
                        t = rnd * 2 + i
                        S.op("pe", lambda e: e.transpose(out=tp[0:16, i * 128:(i + 1) * 128], in_=sel[:, t, :],
                                                         identity=identf[:, :]), reads=[b_g, b_idf], writes=[b_tp])
                    S.op("act", lambda e: e.activation(out=aug[0:16, rnd * 256:(rnd + 1) * 256], in_=tp[0:16, :],
                                                       func=AF.Identity), reads=[b_tp], writes=[b_aug])

            load_kv(0)
            part_A(0); part_B(0); part_D(0)
            for h in range(NH):
                if h + 1 < NH:
                    load_kv(h + 1)
                qt, b_qt = QTh[h % 2]
                aug, b_aug = augTh[h % 2]
                kh, vh, b_kh, b_vh = KV[h % 2]
                gidx = 0
                for half in range(2):
                    hs = slice(half * 512, (half + 1) * 512)
                    Ob, b_Ob = Obs[half]
                    kcs = list(range(24)) + [24 + i for i in range(4 if half == 0 else 8)]
                    nck = len(kcs)
                    sc = {}

                    def emit_scores(idx):
                        kc = kcs[idx]
                        slot_i = kc // 2
                        sp_, b_sp = R[nR[0] % 3]; nR[0] += 1
                        S.op("pe", lambda e: e.matmul(sp_[:, :], lhsT=kh[:, kc * 128:(kc + 1) * 128], rhs=qt[:, hs],
                                                      start=True, stop=False), reads=[b_kh, b_qt], writes=[b_sp])
                        S.op("pe", lambda e: e.matmul(sp_[:, :], lhsT=blksel[0:16, slot_i * 128:(slot_i + 1) * 128],
                                                      rhs=aug[0:16, hs], start=False, stop=True),
                             reads=[b_blksel, b_aug], writes=[b_sp])
                        sc[idx] = (sp_, b_sp)

                    emit_scores(0)
                    emit_scores(1)
                    for idx, kc in enumerate(kcs):
                        sp_, b_sp = sc.pop(idx)
                        pt, b_pt = PTs[npt[0] % 3]; npt[0] += 1
                        S.op("act", lambda e: e.activation(out=pt[:, :], in_=sp_[:, :], func=AF.Exp, scale=float(SCALE)),
                             reads=[b_sp], writes=[b_pt])
                        if kc >= 24:
                            ko = kc - 24
                            kb_ = ko // 2
                            if kb_ // 2 == half:
                                c0 = (kb_ % 2) * 256
                                S.op("pool", lambda e: e.affine_select(out=pt[:, c0:c0 + 256], in_=pt[:, c0:c0 + 256],
                                                                       pattern=[[1, 256]], compare_op=ALU.is_ge,
                                                                       fill=0.0, base=-(ko % 2) * 128,
                                                                       channel_multiplier=-1),
                                     reads=[b_pt], writes=[b_pt])
                        if idx + 2 < nck:
                            emit_scores(idx + 2)
                        last = idx == nck - 1
                        S.op("pe", lambda e: e.matmul(Ob[:, :], lhsT=vh[:, kc, :], rhs=pt[:, :], start=(idx == 0), stop=last),
                             reads=[b_vh, b_pt], writes=[b_Ob])
                        S.op("pe", lambda e: e.matmul(Dn[:, :], lhsT=onesb[:, :], rhs=pt[:, :], start=(idx == 0), stop=last),
                             reads=[b_onesb, b_pt], writes=[b_Dn])
                        gidx += 1
                        if h + 1 < NH:
                            if gidx == 3:
                                part_A(h + 1)
                            elif gidx == 14:
                                part_B(h + 1)
                            elif gidx == 40:
                                part_D(h + 1)
                    S.op("dve", lambda e: e.tensor_scalar(out=rD[:, :], in0=Dn[:, :], scalar1=1e-30, scalar2=None,
                                                          op0=ALU.add), reads=[b_Dn], writes=[b_rD])
                    S.op("dve", lambda e: e.reciprocal(out=rD[:, :], in_=rD[:, :]), reads=[b_rD], writes=[b_rD])
                    S.op("dve", lambda e: e.tensor_tensor(out=attnT[:, h, hs], in0=Ob[:, :], in1=rD[:, :], op=ALU.mult),
                         reads=[b_Ob, b_rD], writes=[b_attnT[h]])
            dump("attnT", attnT[:, :, :], [128, NH, NT], BF16, reads=b_attnT)
            S.barrier()
        if stage <= 3:
            sub1.close()
            finish()
            return nc, dbg_outs

        with ExitStack() as st, ExitStack() as pst:
            hslots = [(ring[s_][0][:, :, hh_ * 256:(hh_ + 1) * 256], Buf()) for s_ in range(3) for hh_ in range(2)]
            mstate2 = {"issued": 0}

            def m_issue_upto(j_end):
                while mstate2["issued"] < min(24, j_end):
                    j = mstate2["issued"]
                    db_, kind = j // 3, j % 3
                    hsl, b_h = hslots[j % 6]
                    c_ = db_ * 256
                    if kind == 0:
                        S.dma("pool", hsl, w_in[:, OFF_GC + c_:OFF_GC + c_ + 256].rearrange("(c p) n -> p c n", p=128),
                              writes=[b_h])
                    elif kind == 1:
                        S.dma("pool", hsl, w_in[:, OFF_GA + c_:OFF_GA + c_ + 256].rearrange("(c p) n -> p c n", p=128),
                              writes=[b_h])
                    else:
                        S.dma("pool", hsl[:, 0:8, :], w_conv_out[:, c_:c_ + 256].rearrange("(c p) n -> p c n", p=128),
                              writes=[b_h])
                        S.dma("pool", hsl[:, 8:16, :], w_attn_out[:, c_:c_ + 256].rearrange("(c p) n -> p c n", p=128),
                              writes=[b_h])
                    mstate2["issued"] += 1

            bk = [(ps(pst, f"mb{i}", [128, 512]), Buf()) for i in range(8)]
            tmpf = [(sb(st, f"mt{i}", [128, 512], F32), Buf()) for i in range(8)]
            nbk_ = 0
            m_issue_upto(3)
            for db in range(8):
                m_issue_upto(3 * (db + 2))
                gcs, b_gcs = hslots[(3 * db) % 6]
                gas, b_gas = hslots[(3 * db + 1) % 6]
                cas, b_cas = hslots[(3 * db + 2) % 6]
                for dc in range(2):
                    dch = db * 2 + dc
                    cols = slice(dc * 128, (dc + 1) * 128)
                    for half in range(2):
                        hs = slice(half * 512, (half + 1) * 512)
                        (gcb, b_gcb), (gab, b_gab), (ycb, b_ycb), (yab, b_yab) = [bk[(nbk_ + i) % 8] for i in range(4)]
                        (sgc, b_sgc), (sga, b_sga), (t1, b_t1), (t2, b_t2) = [tmpf[(nbk_ + i) % 8] for i in range(4)]
                        nbk_ += 4
                        for k in range(16):
                            S.op("pe", lambda e: e.matmul(gcb[:, :], lhsT=gcs[:, k, cols], rhs=uT[:, k, hs],
                                                          start=(k == 0), stop=(k == 15)),
                                 reads=[b_gcs, b_uT[half]], writes=[b_gcb])
                        for k in range(16):
                            S.op("pe", lambda e: e.matmul(gab[:, :], lhsT=gas[:, k, cols], rhs=uT[:, k, hs],
                                                          start=(k == 0), stop=(k == 15)),
                                 reads=[b_gas, b_uT[half]], writes=[b_gab])
                        for k in range(8):
                            S.op("pe", lambda e: e.matmul(ycb[:, :], lhsT=cas[:, k, cols], rhs=sT[:, k, hs],
                                                          start=(k == 0), stop=(k == 7)),
                                 reads=[b_cas, b_sT], writes=[b_ycb])
                        for k in range(8):
                            S.op("pe", lambda e: e.matmul(yab[:, :], lhsT=cas[:, 8 + k, cols], rhs=attnT[:, k, hs],
                                                          start=(k == 0), stop=(k == 7)),
                                 reads=[b_cas, b_attnT[k]], writes=[b_yab])
                        S.op("act", lambda e: e.activation(out=sgc[:, :], in_=gcb[:, :], func=AF.Sigmoid),
                             reads=[b_gcb], writes=[b_sgc])
                        S.op("act", lambda e: e.activation(out=sga[:, :], in_=gab[:, :], func=AF.Sigmoid),
                             reads=[b_gab], writes=[b_sga])
                        S.op("dve", lambda e: e.scalar_tensor_tensor(out=t1[:, :], in0=ycb[:, :], scalar=V("bco", dch),
                                                                     in1=sgc[:, :], op0=ALU.add, op1=ALU.mult),
                             reads=[b_ycb, b_sgc, b_vecs], writes=[b_t1])
                        S.op("dve", lambda e: e.tensor_tensor(out=t2[:, :], in0=yab[:, :], in1=sga[:, :], op=ALU.mult),
                             reads=[b_yab, b_sga], writes=[b_t2])
                        S.op("dve", lambda e: e.tensor_tensor(out=mu[:, dch, hs], in0=t1[:, :], in1=t2[:, :], op=ALU.add),
                             reads=[b_t1, b_t2], writes=[b_mu[half]])
            dump("mergedT", mu[:, :, :], [128, 16, NT], BF16, reads=b_mu)
            S.barrier()
        sub1.close()
        if stage <= 4:
            finish()
            return nc, dbg_outs

        stB = ExitStack()
        big = sb(stB, "big", [128, 16, NT], F32); b_big = [Buf() for _ in range(16)]
        gateT = sb(stB, "gateT", [32, NT], F32); b_gateT = Buf()
        bg = lambda c, half: big[:, c, half * 512:(half + 1) * 512]
        with ExitStack() as st, ExitStack() as pst:
            xs = [(sb(st, f"xs{i}", [128, 8, 512], F32), Buf()) for i in range(1)]
            tgt = [(sb(st, f"tg{i}", [128, 512], F32), Buf()) for i in range(1)]
            tb = [(ps(pst, f"tb{i}", [128, 512]), Buf()) for i in range(2)]
            xTb = [(ps(pst, f"xTb{i}", [128, 512]), Buf()) for i in range(2)]
            mp = ps(pst, "mp1", [128, 512]); b_mp = Buf()
            ep = ps(pst, "ep1", [128, 512]); b_ep = Buf()
            lpb = ps(pst, "lpb", [128, 288]); b_lp = Buf()
            gtpb = ps(pst, "gtpb", [32, 512]); b_gtp = Buf()
            sqr = [(sb(st, f"sq1{i}", [128, 512], F32), Buf()) for i in range(2)]
            msq = sb(st, "msq1", [128, 512], F32); b_msq = Buf()
            meanb = sb(st, "meanb1", [128, NT], F32)
            rstdb = sb(st, "rstdb1", [128, NT], F32); b_stat = Buf()
            ws = WStream([colblock(w_mix_out, db * 512) for db in range(4)], nslots=4, lookahead=1)
            n2 = 0
            for db in range(4):
                slot, bslot = ws.get(db)
                xsl, b_xsl = xs[0]
                S.dma("sp", xsl[:, :, :], x_own[:, db * 512:(db + 1) * 512].rearrange("(t p) n -> p t n", p=128),
                      writes=[b_xsl])
                for dc in range(4):
                    dch = db * 4 + dc
                    for half in range(2):
                        hs = slice(half * 512, (half + 1) * 512)
                        tbk, b_tbk = tb[n2 % 2]
                        xb, b_xb = xTb[n2 % 2]
                        tg, b_tg = tgt[0]
                        n2 += 1
                        for k in range(16):
                            S.op("pe", lambda e: e.matmul(tbk[:, :], lhsT=slot[:, k, dc * 128:(dc + 1) * 128],
                                                          rhs=mu[:, k, hs], start=(k == 0), stop=(k == 15)),
                                 reads=[bslot, b_mu[half]], writes=[b_tbk])
                        for tt in range(4):
                            S.op("pe", lambda e: e.transpose(out=xb[:, tt * 128:(tt + 1) * 128],
                                                             in_=xsl[:, half * 4 + tt, dc * 128:(dc + 1) * 128],
                                                             identity=identf[:, :]),
                                 reads=[b_xsl, b_idf], writes=[b_xb])
                        S.op("act", lambda e: e.activation(out=tg[:, :], in_=tbk[:, :], func=AF.Identity,
                                                           scale=opm[:, GT1 + dch:GT1 + dch + 1]),
                             reads=[b_tbk, b_opm], writes=[b_tg])
                        S.op("dve", lambda e: e.scalar_tensor_tensor(out=bg(dch, half), in0=xb[:, :], scalar=float(ALPHA),
                                                                     in1=tg[:, :], op0=ALU.mult, op1=ALU.add),
                             reads=[b_xb, b_tg], writes=[b_big[dch]])
            ln_stats(bg, lambda c: b_big[c], 16, onesD, b_onesD, meanb, rstdb, b_stat, sqr, mp, b_mp, ep, b_ep, msq, b_msq)
            for c in range(16):
                for half in range(2):
                    hs = slice(half * 512, (half + 1) * 512)
                    S.op("dve",
                         lambda e: e.tensor_tensor(out=bg(c, half), in0=bg(c, half), in1=meanb[:, hs],
                                                   op=ALU.subtract), reads=[b_big[c], b_stat], writes=[b_big[c]])
                    S.op("dve", lambda e: e.scalar_tensor_tensor(out=bg(c, half), in0=bg(c, half), scalar=V("gln1", c),
                                                                 in1=rstdb[:, hs], op0=ALU.mult, op1=ALU.mult),
                         reads=[b_big[c], b_stat, b_vecs], writes=[b_big[c]])
                    S.op("act", lambda e: e.activation(out=bg(c, half), in_=bg(c, half), func=AF.Identity,
                                                       bias=V("bln1", c)), reads=[b_big[c], b_vecs], writes=[b_big[c]])
            b_x1d = Buf()
            S.dma("sp", x1_d, big[:].rearrange("p a b -> p (a b)"), reads=b_big, writes=[b_x1d])
            dump("x1T", big[:, :, :], [128, 16, NT], F32, reads=b_big)
            ln_stats(bg, lambda c: b_big[c], 16, onesD, b_onesD, meanb, rstdb, b_stat, sqr, mp, b_mp, ep, b_ep, msq, b_msq)
            for c in range(16):
                for half in range(2):
                    hs = slice(half * 512, (half + 1) * 512)
                    S.op("dve",
                         lambda e: e.tensor_tensor(out=bg(c, half), in0=bg(c, half), in1=meanb[:, hs],
                                                   op=ALU.subtract), reads=[b_big[c], b_stat], writes=[b_big[c]])
                    S.op("dve", lambda e: e.scalar_tensor_tensor(out=bg(c, half), in0=bg(c, half),
                                                                 scalar=opm[:, SC2 + c:SC2 + c + 1],
                                                                 in1=rstdb[:, hs], op0=ALU.mult, op1=ALU.mult),
                         reads=[b_big[c], b_stat, b_opm], writes=[b_big[c]])
                    S.op("act", lambda e: e.activation(out=bg(c, half), in_=bg(c, half), func=AF.Identity,
                                                       bias=modT[:, SH2 + c:SH2 + c + 1]),
                         reads=[b_big[c], b_modT], writes=[b_big[c]])
                    S.op("act", lambda e: e.activation(out=mu[:, c, hs], in_=bg(c, half), func=AF.Identity),
                         reads=[b_big[c]], writes=[b_mu[half]])
            dump("u2T", big[:, :, :], [128, 16, NT], F32, reads=b_big)
            wr = sb(st, "wr", [128, 16 * 36], F32); b_wr = Buf()
            brow = sb(st, "brow", [128, 36], F32); b_brow = Buf()
            S.dma("sp", wr[:], wr_in[:, :], writes=[b_wr])
            S.dma("sp", brow[:], brow_in[:, :], writes=[b_brow])
            b_r = Buf()
            lg = sb(st, "lg", [128, 8, 36], F32)
            gmx = sb(st, "gmx", [128, 8], F32)
            goh = sb(st, "goh", [128, 8, 4], F32)
            exg = sb(st, "exg", [128, 8, 4], F32)
            sume = sb(st, "sume", [128, 8], F32)
            ptop = sb(st, "ptop", [128, 8], F32)
            ig = sb(st, "ig", [128, 8, 8], F32)
            tm8 = sb(st, "tm8", [128, 8, 8], F32)
            ig8 = sb(st, "ig8", [128, 8, 8], F32)
            dlt = sb(st, "dlt", [128, 8], F32)
            w1p = sb(st, "w1p", [128, 8], F32)
            w2p = sb(st, "w2p", [128, 8], F32)
            e1 = sb(st, "e1", [128, 8, 8], F32)
            e2 = sb(st, "e2", [128, 8, 8], F32)
            g32 = sb(st, "g32", [128, 8, 32], F32)
            for t in range(8):
                ts_ = slice(t * 128, (t + 1) * 128)
                for c in range(16):
                    S.op("pe", lambda e: e.matmul(lpb[:, t * 36:(t + 1) * 36], lhsT=big[:, c, ts_], rhs=wr[:, c * 36:(c + 1) * 36],
                                                  start=(c == 0), stop=(c == 15)), reads=[b_big[c], b_wr], writes=[b_lp])
            B3 = lambda ap, n: ap.unsqueeze(2).to_broadcast([128, 8, n])
            S.op("dve", lambda e: e.tensor_tensor(out=lg[:], in0=lpb[:, 0:288].rearrange("p (a b) -> p a b", b=36),
                                                  in1=brow[:, :].unsqueeze(1).to_broadcast([128, 8, 36]), op=ALU.add),
                 reads=[b_lp, b_brow], writes=[b_r])
            S.op("dve", lambda e: e.tensor_reduce(out=gmx[:], in_=lg[:, :, 0:4], axis=AX, op=ALU.max),
                 reads=[b_r], writes=[b_r])
            S.op("dve", lambda e: e.tensor_tensor(out=goh[:], in0=lg[:, :, 0:4], in1=B3(gmx[:], 4), op=ALU.is_ge),
                 reads=[b_r], writes=[b_r])
            S.op("dve", lambda e: e.tensor_tensor(out=exg[:], in0=lg[:, :, 0:4], in1=B3(gmx[:], 4), op=ALU.subtract),
                 reads=[b_r], writes=[b_r])
            S.op("act", lambda e: e.activation(out=exg[:], in_=exg[:], func=AF.Exp), reads=[b_r], writes=[b_r])
            S.op("dve", lambda e: e.tensor_reduce(out=sume[:], in_=exg[:], axis=AX, op=ALU.add), reads=[b_r], writes=[b_r])
            S.op("dve", lambda e: e.reciprocal(out=ptop[:], in_=sume[:]), reads=[b_r], writes=[b_r])
            for g_ in range(4):
                dst_ = ig if g_ == 0 else tm8
                S.op("dve", lambda e: e.tensor_tensor(out=dst_[:], in0=lg[:, :, 4 + 8 * g_:12 + 8 * g_],
                                                      in1=goh[:, :, g_:g_ + 1].to_broadcast([128, 8, 8]), op=ALU.mult),
                     reads=[b_r], writes=[b_r])
                if g_ > 0:
                    S.op("dve", lambda e: e.tensor_tensor(out=ig[:], in0=ig[:], in1=tm8[:], op=ALU.add),
                         reads=[b_r], writes=[b_r])
            for t in range(8):
                S.op("dve", lambda e: e.max(out=ig8[:, t, :], in_=ig[:, t, :]), reads=[b_r], writes=[b_r])
            S.op("dve", lambda e: e.tensor_tensor(out=dlt[:], in0=ig8[:, :, 1], in1=ig8[:, :, 0], op=ALU.subtract),
                 reads=[b_r], writes=[b_r])
            S.op("act", lambda e: e.activation(out=dlt[:], in_=dlt[:], func=AF.Exp), reads=[b_r], writes=[b_r])
            S.op("dve", lambda e: e.tensor_scalar(out=dlt[:], in0=dlt[:], scalar1=1.0, scalar2=None, op0=ALU.add),
                 reads=[b_r], writes=[b_r])
            S.op("dve", lambda e: e.reciprocal(out=w1p[:], in_=dlt[:]), reads=[b_r], writes=[b_r])
            S.op("dve", lambda e: e.tensor_tensor(out=w1p[:], in0=w1p[:], in1=ptop[:], op=ALU.mult), reads=[b_r], writes=[b_r])
            S.op("dve", lambda e: e.tensor_tensor(out=w2p[:], in0=ptop[:], in1=w1p[:], op=ALU.subtract),
                 reads=[b_r], writes=[b_r])
            S.op("dve", lambda e: e.tensor_tensor(out=e1[:], in0=ig[:], in1=ig8[:, :, 0:1].to_broadcast([128, 8, 8]),
                                                  op=ALU.is_equal), reads=[b_r], writes=[b_r])
            S.op("dve", lambda e: e.tensor_tensor(out=e1[:], in0=e1[:], in1=B3(w1p[:], 8), op=ALU.mult),
                 reads=[b_r], writes=[b_r])
            S.op("dve", lambda e: e.tensor_tensor(out=e2[:], in0=ig[:], in1=ig8[:, :, 1:2].to_broadcast([128, 8, 8]),
                                                  op=ALU.is_equal), reads=[b_r], writes=[b_r])
            S.op("dve", lambda e: e.tensor_tensor(out=e2[:], in0=e2[:], in1=B3(w2p[:], 8), op=ALU.mult),
                 reads=[b_r], writes=[b_r])
            S.op("dve", lambda e: e.tensor_tensor(out=e1[:], in0=e1[:], in1=e2[:], op=ALU.add), reads=[b_r], writes=[b_r])
            for g_ in range(4):
                S.op("dve", lambda e: e.tensor_tensor(out=g32[:, :, 8 * g_:8 * g_ + 8], in0=e1[:],
                                                      in1=goh[:, :, g_:g_ + 1].to_broadcast([128, 8, 8]), op=ALU.mult),
                     reads=[b_r], writes=[b_r])
            for rnd in range(2):
                for i in range(4):
                    t = rnd * 4 + i
                    S.op("pe", lambda e: e.transpose(out=gtpb[0:32, i * 128:(i + 1) * 128], in_=g32[:, t, :],
                                                     identity=identf[:, :]), reads=[b_r, b_idf], writes=[b_gtp])
                S.op("act", lambda e: e.activation(out=gateT[0:32, rnd * 512:(rnd + 1) * 512], in_=gtpb[0:32, :],
                                                   func=AF.Identity), reads=[b_gtp], writes=[b_gateT])
            b_gated = Buf()
            S.dma("sp", gate_d, gateT[:, :], reads=[b_gateT], writes=[b_gated])
            dump("lg", lg[:, :, :], [128, 8, 36], F32, reads=[b_r])
            dump("gateT", gateT[:, :], [32, NT], F32, reads=[b_gateT])
            S.barrier()
        if stage <= 5:
            stB.close()
            finish()
            return nc, dbg_outs

        with ExitStack() as st, ExitStack() as pst:
            gTs = [(sb(st, f"gT{i}", [128, 4, NT], BF16), Buf()) for i in range(2)]
            gbs = [(sb(st, f"gb{i}", [128, NT], F32), Buf()) for i in range(2)]
            s1s = [(sb(st, f"s1{i}", [128, 512], F32), Buf()) for i in range(2)]
            ggs = [(sb(st, f"gg{i}", [128, 512], F32), Buf()) for i in range(2)]
            hb = [(ps(pst, f"hb{i}", [128, 512]), Buf()) for i in range(4)]
            yb = [(ps(pst, f"yb{i}", [128, 512]), Buf()) for i in range(3)]
            blocks = []
            for e_ in range(NEXP):
                blocks.append(colblock(w1[e_], 0))
                blocks.append(colblock(w3[e_], 0))
                blocks.append([(lambda s: s[:].rearrange("p (f a) b -> p f (a b)", f=4),
                                w2[e_].rearrange("(f p) d -> p f d", p=128))])
            ws = WStream(blocks, nslots=4, lookahead=1)
            nh = 0
            ny = 0
            for e_ in range(NEXP):
                w1s, b_w1s = ws.get(3 * e_)
                w3s, b_w3s = ws.get(3 * e_ + 1)
                w2s_, b_w2s = ws.get(3 * e_ + 2)
                w2s = w2s_[:].rearrange("p (f a) b -> p f (a b)", f=4)
                gb, b_gb = gbs[e_ % 2]
                S.dma("sp", gb[:, :], gate_d[e_:e_ + 1, :].to_broadcast([128, NT]), writes=[b_gb])
                gT, b_gT = gTs[e_ % 2]
                for fc in range(4):
                    for half in range(2):
                        hs = slice(half * 512, (half + 1) * 512)
                        h1b, b_h1b = hb[nh % 4]; nh += 1
                        h3b, b_h3b = hb[nh % 4]; nh += 1
                        for k in range(16):
                            S.op("pe", lambda e: e.matmul(h1b[:, :], lhsT=w1s[:, k, fc * 128:(fc + 1) * 128], rhs=mu[:, k, hs],
                                                          start=(k == 0), stop=(k == 15)),
                                 reads=[b_w1s, b_mu[half]], writes=[b_h1b])
                        for k in range(16):
                            S.op("pe", lambda e: e.matmul(h3b[:, :], lhsT=w3s[:, k, fc * 128:(fc + 1) * 128], rhs=mu[:, k, hs],
                                                          start=(k == 0), stop=(k == 15)),
                                 reads=[b_w3s, b_mu[half]], writes=[b_h3b])
                        s1, b_s1 = s1s[(nh // 2) % 2]
                        gg, b_gg = ggs[(nh // 2) % 2]
                        S.op("act", lambda e: e.activation(out=s1[:, :], in_=h1b[:, :], func=AF.Silu),
                             reads=[b_h1b], writes=[b_s1])
                        S.op("dve", lambda e: e.tensor_tensor(out=gg[:, :], in0=s1[:, :], in1=h3b[:, :], op=ALU.mult),
                             reads=[b_s1, b_h3b], writes=[b_gg])
                        S.op("dve", lambda e: e.tensor_tensor(out=gT[:, fc, hs], in0=gg[:, :], in1=gb[:, hs], op=ALU.mult),
                             reads=[b_gg, b_gb], writes=[b_gT])
                for dc in range(16):
                    for half in range(2):
                        hs = slice(half * 512, (half + 1) * 512)
                        ybk, b_ybk = yb[ny % 3]; ny += 1
                        for fc in range(4):
                            S.op("pe", lambda e: e.matmul(ybk[:, :], lhsT=w2s[:, fc, dc * 128:(dc + 1) * 128], rhs=gT[:, fc, hs],
                                                          start=(fc == 0), stop=(fc == 3)),
                                 reads=[b_w2s, b_gT], writes=[b_ybk])
                        if e_ == 0:
                            S.op("act", lambda e: e.activation(out=bg(dc, half), in_=ybk[:, :], func=AF.Identity),
                                 reads=[b_ybk], writes=[b_big[dc]])
                        else:
                            S.op("dve", lambda e: e.tensor_tensor(out=bg(dc, half), in0=bg(dc, half), in1=ybk[:, :],
                                                                  op=ALU.add), reads=[b_ybk, b_big[dc]], writes=[b_big[dc]])
            dump("fT", big[:, :, :], [128, 16, NT], F32, reads=b_big)
            S.barrier()

        with ExitStack() as st, ExitStack() as pst:
            x1c = [(sb(st, f"x1c{i}", [128, 512], F32), Buf()) for i in range(4)]
            mp = ps(pst, "mp2", [128, 512]); b_mp = Buf()
            ep = ps(pst, "ep2", [128, 512]); b_ep = Buf()
            ob = [(ps(pst, f"ob{i}", [128, 512]), Buf()) for i in range(3)]
            sqr = [(sb(st, f"sq2{i}", [128, 512], F32), Buf()) for i in range(2)]
            msq = sb(st, "msq2", [128, 512], F32); b_msq = Buf()
            meanb = sb(st, "meanb2", [128, NT], F32)
            rstdb = sb(st, "rstdb2", [128, NT], F32); b_stat = Buf()
            otile = [(sb(st, f"ot{i}", [128, D], F32), Buf()) for i in range(2)]
            n3 = 0
            for c in range(16):
                for half in range(2):
                    xc, b_xc = x1c[n3 % 4]; n3 += 1
                    S.dma("sp", xc[:, :], x1_d[:, c * NT + half * 512:c * NT + (half + 1) * 512], reads=[b_x1d], writes=[b_xc])
                    S.op("act", lambda e: e.activation(out=xc[:, :], in_=xc[:, :], func=AF.Identity, scale=float(ALPHA)),
                         reads=[b_xc], writes=[b_xc])
                    S.op("dve", lambda e: e.scalar_tensor_tensor(out=bg(c, half), in0=bg(c, half),
                                                                 scalar=opm[:, GT2 + c:GT2 + c + 1], in1=xc[:, :],
                                                                 op0=ALU.mult, op1=ALU.add),
                         reads=[b_big[c], b_xc, b_opm], writes=[b_big[c]])
            ln_stats(bg, lambda c: b_big[c], 16, onesD, b_onesD, meanb, rstdb, b_stat, sqr, mp, b_mp, ep, b_ep, msq, b_msq)
            for c in range(16):
                for half in range(2):
                    hs = slice(half * 512, (half + 1) * 512)
                    S.op("dve",
                         lambda e: e.tensor_tensor(out=bg(c, half), in0=bg(c, half), in1=meanb[:, hs],
                                                   op=ALU.subtract), reads=[b_big[c], b_stat], writes=[b_big[c]])
                    S.op("dve", lambda e: e.scalar_tensor_tensor(out=bg(c, half), in0=bg(c, half), scalar=V("gln2", c),
                                                                 in1=rstdb[:, hs], op0=ALU.mult, op1=ALU.mult),
                         reads=[b_big[c], b_stat, b_vecs], writes=[b_big[c]])
                    S.op("act", lambda e: e.activation(out=bg(c, half), in_=bg(c, half), func=AF.Identity,
                                                       bias=V("bln2", c)), reads=[b_big[c], b_vecs], writes=[b_big[c]])
            no = 0
            for t in range(8):
                ot, b_ot = otile[t % 2]
                for cg in range(4):
                    obk, b_obk = ob[no % 3]; no += 1
                    for i in range(4):
                        c = cg * 4 + i
                        S.op("pe", lambda e: e.transpose(out=obk[:, i * 128:(i + 1) * 128], in_=big[:, c, t * 128:(t + 1) * 128],
                                                         identity=identf[:, :]), reads=[b_big[c], b_idf], writes=[b_obk])
                    if cg % 2 == 0:
                        S.op("act", lambda e: e.activation(out=ot[:, cg * 512:(cg + 1) * 512], in_=obk[:, :], func=AF.Identity),
                             reads=[b_obk], writes=[b_ot])
                    else:
                        S.op("dve", lambda e: e.tensor_copy(out=ot[:, cg * 512:(cg + 1) * 512], in_=obk[:, :]),
                             reads=[b_obk], writes=[b_ot])
                S.dma("sp", out[t * 128:(t + 1) * 128, :], ot[:, :], reads=[b_ot], writes=[Buf()])
        stB.close()
        finish()
    return nc, dbg_outs


def _fm(v, nch):
    return np.ascontiguousarray(np.asarray(v, np.float32).reshape(nch, 128).T)


def make_in_maps(inputs):
    x = np.asarray(inputs["x"], np.float32)
    c = np.asarray(inputs["c"], np.float32)
    positions = np.asarray(inputs["positions"], np.int32)
    g = lambda n: np.asarray(inputs[n])[0]
    half = 8
    invf = (500000.0 ** (-np.arange(half, dtype=np.float32) / np.float32(half))).astype(np.float32)
    invf16 = np.zeros(16, np.float32)
    invf16 = np.power(np.float32(500000.0), -np.arange(16, dtype=np.float32) / np.float32(16)).astype(np.float32)
    prot = np.zeros((32, 32), np.float32)
    for m in range(16):
        prot[m + 16, m] = -1.0
        prot[m, m + 16] = 1.0
    blksel = np.zeros((16, 16, 128), np.float32)
    for s in range(16):
        blksel[s, s, :] = 1.0
    blksel = blksel.reshape(16, 16 * 128)
    wdw = g("w_dw")
    wdwT = np.ascontiguousarray(wdw.reshape(CW, 8, 128).transpose(2, 1, 0).reshape(128, 8 * CW)).astype(np.float32)
    wr_full = np.concatenate([g("w_grp"), g("w_erouter")], axis=1)
    wr = np.ascontiguousarray(wr_full.reshape(16, 128, 36).transpose(1, 0, 2).reshape(128, 16 * 36)).astype(np.float32)
    brow = np.tile(np.concatenate([g("b_grp"), g("b_erouter")])[None, :], (128, 1)).astype(np.float32)
    shared = {
        "wdwT": wdwT, "brow": brow, "wr": wr, "prot": prot, "blksel": blksel,
        "w_cond": np.ascontiguousarray(g("w_cond")), "w_in": np.ascontiguousarray(g("w_in")),
        "w_conv_out": np.ascontiguousarray(g("w_conv_out")), "w_attn_out": np.ascontiguousarray(g("w_attn_out")),
        "w_mix_out": np.ascontiguousarray(g("w_mix_out")),
        "w1": np.ascontiguousarray(g("w1").reshape(NEXP, D, FF)),
        "w3": np.ascontiguousarray(g("w3").reshape(NEXP, D, FF)),
        "w2": np.ascontiguousarray(g("w2").reshape(NEXP, FF, D)),
    }
    maps = []
    for core in range(8):
        b, j = core // 4, core % 4
        s0 = j * NT
        vec = np.zeros((128, NV), np.float32)

        def put(name, arr):
            o, w = VOFF[name]
            vec[:, o:o + w] = arr
        put("bglu", _fm(g("b_glu"), 16)); put("bdw", _fm(g("b_dw"), 8)); put("gcn", _fm(g("g_cn"), 8))
        put("bcn", _fm(g("b_cn"), 8)); put("bco", _fm(g("b_conv_out"), 16)); put("gln1", _fm(g("g_ln1"), 16))
        put("bln1", _fm(g("b_ln1"), 16)); put("gln2", _fm(g("g_ln2"), 16)); put("bln2", _fm(g("b_ln2"), 16))
        put("bcond", _fm(g("b_cond"), 96)); put("c", _fm(c[b], 16))
        put("halo", np.full((128, 1), 1.0 if j > 0 else 0.0, np.float32))
        iv = np.zeros((128, 1), np.float32)
        iv[0:32, 0] = np.concatenate([invf16, invf16])
        put("invf", iv)
        if j > 0:
            x_halo = x[b, s0 - 128:s0]
        else:
            x_halo = np.zeros((128, D), np.float32)
        pos = np.concatenate([positions[b, 0:NCTX], positions[b, s0:s0 + NT]]).astype(np.int32)
        negmask = np.zeros((8, 16), np.float32)
        force = np.zeros((8, 16), np.float32)
        for tq in range(8):
            lb = tq // 2
            for s in range(12):
                negmask[tq, s] = 0.0 if s < 4 * j else -1e30
            for i in range(4):
                negmask[tq, 12 + i] = 0.0 if i < lb else -1e30
            force[tq, 12 + lb] = 1.0
        m = dict(shared)
        m.update({
            "x_own": np.ascontiguousarray(x[b, s0:s0 + NT]),
            "x_halo": np.ascontiguousarray(x_halo),
            "x_ctx": np.ascontiguousarray(x[b, 0:NCTX]),
            "pos": np.ascontiguousarray(np.tile(pos[None, :], (32, 1))),
            "vecs": vec,
            "negmask": np.tile(negmask.reshape(1, 128), (128, 1)).astype(np.float32),
            "force": np.tile(force.reshape(1, 128), (128, 1)).astype(np.float32),
        })
        maps.append(m)
    return maps


_CACHE = {}


def kernel(**inputs):
    if "nc" not in _CACHE:
        _CACHE["nc"] = build_program()[0]
    nc = _CACHE["nc"]
    maps = make_in_maps(inputs)
    res = run_bass_kernel_spmd(nc, maps, core_ids=list(range(8)))
    outp = np.zeros((2, SEQ, D), np.float32)
    for core in range(8):
        b, j = core // 4, core % 4
        outp[b, j * NT:(j + 1) * NT] = res.results[core]["out"]
    return outp
```
